# Optimizing a Trainium2 kernel written in Bass

```python
import math
import jax
import jax.numpy as jnp
from jax import lax
import numpy as np

D_MODEL = 2048
BATCH = 4
SEQ = 2048
DEPTH = 2

PLE_DIM = 256
ROPE_THETA = 500000.0
NORM_EPS = 1e-6
ATTN_BLOCK = 128

CONV_DIM = 448
CONV_K = 3

SSM_DIM = 448
SSM_GROUP = 16
SSM_GROUPS = SSM_DIM // SSM_GROUP
SSM_STATE = 64

DIL_HEAD_DIM = 64
DIL_ROT = DIL_HEAD_DIM // 4
DIL_PATTERNS = ((128, 1), (512, 4), (2048, 16))
DIL_HEADS_PER_GROUP = 3
DIL_HEADS = DIL_HEADS_PER_GROUP * len(DIL_PATTERNS)
DIL_DIM = DIL_HEADS * DIL_HEAD_DIM

MLA_HEADS = 9
MLA_Q_RANK = 384
MLA_KV_RANK = 256
MLA_NOPE = 64
MLA_ROPE = 32
MLA_V = 64
MLA_QK = MLA_NOPE + MLA_ROPE
MLA_DIM = MLA_HEADS * MLA_V

D_MIX = CONV_DIM + SSM_DIM + DIL_DIM + MLA_DIM
IN_SPLITS = (CONV_DIM, CONV_DIM, CONV_DIM, SSM_DIM, DIL_DIM, DIL_DIM, DIL_DIM, MLA_Q_RANK, MLA_KV_RANK, MLA_ROPE)
D_IN = sum(IN_SPLITS)

MOE_GROUPS = 8
MOE_EPG = 8
N_EXPERTS = MOE_GROUPS * MOE_EPG
MOE_TOPK = 2
MOE_FF = 512
MOE_BLOCK = 128

kernel_name = 'hymba_style_hybrid_hmoe_trunk'


def rmsnorm(x, g):
    xf = x.astype(jnp.float32)
    y = xf * lax.rsqrt(jnp.mean(xf * xf, axis=-1, keepdims=True) + NORM_EPS)
    return (y * g.astype(jnp.float32)).astype(x.dtype)


def rope_cos_sin(positions, rot_dim):
    inv_freq = ROPE_THETA ** (-jnp.arange(0, rot_dim, 2, dtype=jnp.float32) / rot_dim)
    ang = positions.astype(jnp.float32)[..., None] * inv_freq
    return jnp.cos(ang), jnp.sin(ang)


def apply_rope(x, cos, sin):
    half = cos.shape[-1]
    c = cos[:, :, None, :].astype(x.dtype)
    s = sin[:, :, None, :].astype(x.dtype)
    x1 = x[..., :half]
    x2 = x[..., half:2 * half]
    return jnp.concatenate([x1 * c - x2 * s, x2 * c + x1 * s, x[..., 2 * half:]], axis=-1)


def short_conv_mixer(b, c, h, conv_w):
    u = c * h
    y = lax.conv_general_dilated(u, conv_w[:, None, :].astype(u.dtype), window_strides=(1,),
                                 padding=((CONV_K - 1, 0),), dimension_numbers=('NWC', 'WIO', 'NWC'),
                                 feature_group_count=CONV_DIM)
    return b * y


def s5_mixer(u, lam_re, lam_im, log_dt, b_re, b_im, c_re, c_im, d_skip, glu_w, glu_b):
    Bn, S, _ = u.shape
    f32 = jnp.float32
    uf = u.astype(f32)
    ug = uf.reshape(Bn, S, SSM_GROUPS, SSM_GROUP)
    lr = lam_re.astype(f32)
    li = lam_im.astype(f32)
    dt = jnp.exp(log_dt.astype(f32))[:, None]
    mag = jnp.exp(lr * dt)
    lbar_re = mag * jnp.cos(li * dt)
    lbar_im = mag * jnp.sin(li * dt)
    nr = lbar_re - 1.0
    ni = lbar_im
    den = lr * lr + li * li
    f_re = (nr * lr + ni * li) / den
    f_im = (ni * lr - nr * li) / den
    bu_re = jnp.einsum('bsgc,gpc->bsgp', ug, b_re.astype(f32))
    bu_im = jnp.einsum('bsgc,gpc->bsgp', ug, b_im.astype(f32))
    in_re = f_re * bu_re - f_im * bu_im
    in_im = f_re * bu_im + f_im * bu_re
    a_re = jnp.broadcast_to(lbar_re, in_re.shape)
    a_im = jnp.broadcast_to(lbar_im, in_im.shape)

    def combine(e1, e2):
        a1r, a1i, b1r, b1i = e1
        a2r, a2i, b2r, b2i = e2
        return (a2r * a1r - a2i * a1i, a2r * a1i + a2i * a1r,
                a2r * b1r - a2i * b1i + b2r, a2r * b1i + a2i * b1r + b2i)

    _, _, xr, xi = lax.associative_scan(combine, (a_re, a_im, in_re, in_im), axis=1)
    y = (jnp.einsum('bsgp,gcp->bsgc', xr, c_re.astype(f32))
         - jnp.einsum('bsgp,gcp->bsgc', xi, c_im.astype(f32)))
    y = y.reshape(Bn, S, SSM_DIM) + d_skip.astype(f32) * uf
    g = jax.nn.gelu(y)
    out = g * jax.nn.sigmoid(g @ glu_w.astype(f32) + glu_b.astype(f32))
    return out.astype(u.dtype)


def banded_causal_attention(q, k, v, steps):
    N, L, H, hd = q.shape
    blk = steps
    nb = -(-L // blk)
    Lp = nb * blk
    pad = Lp - L
    scale = 1.0 / math.sqrt(hd)
    qp = jnp.pad(q, ((0, 0), (0, pad), (0, 0), (0, 0)))
    kp = jnp.pad(k, ((0, 0), (blk, pad), (0, 0), (0, 0)))
    vp = jnp.pad(v, ((0, 0), (blk, pad), (0, 0), (0, 0)))
    qb = qp.reshape(N, nb, blk, H, hd)
    kb = jnp.concatenate([kp[:, :Lp].reshape(N, nb, blk, H, hd), kp[:, blk:].reshape(N, nb, blk, H, hd)], axis=2)
    vb = jnp.concatenate([vp[:, :Lp].reshape(N, nb, blk, H, hd), vp[:, blk:].reshape(N, nb, blk, H, hd)], axis=2)
    s = jnp.einsum('nbqhd,nbkhd->nbhqk', qb, kb).astype(jnp.float32) * scale
    qi = jnp.arange(blk)[:, None]
    ki = jnp.arange(2 * blk)[None, :]
    dist = qi + blk - ki
    key_abs = jnp.arange(nb)[:, None, None] * blk - blk + ki[None]
    valid = (dist >= 0)[None] & (dist <= steps)[None] & (key_abs >= 0)
    s = jnp.where(valid[None, :, None], s, -jnp.inf)
    lse = jax.nn.logsumexp(s, axis=-1)
    pr = jnp.exp(s - lse[..., None]).astype(v.dtype)
    o = jnp.einsum('nbhqk,nbkhd->nbqhd', pr, vb).reshape(N, Lp, H, hd)[:, :L]
    lse = lse.transpose(0, 1, 3, 2).reshape(N, Lp, H)[:, :L]
    return o, lse


def dilated_group_attention(q, k, v, dil, steps):
    Bn, S, H, hd = q.shape
    L = S // dil

    def to_res(t):
        return t.reshape(Bn, L, dil, H, hd).transpose(0, 2, 1, 3, 4).reshape(Bn * dil, L, H, hd)

    o, lse = banded_causal_attention(to_res(q), to_res(k), to_res(v), steps)
    o = o.reshape(Bn, dil, L, H, hd).transpose(0, 2, 1, 3, 4).reshape(Bn, S, H, hd)
    lse = lse.reshape(Bn, dil, L, H).transpose(0, 2, 1, 3).reshape(Bn, S, H)
    return o, lse


def dilated_mixer(q, k, v, cos, sin):
    Bn, S, _ = q.shape
    q = apply_rope(q.reshape(Bn, S, DIL_HEADS, DIL_HEAD_DIM), cos, sin)
    k = apply_rope(k.reshape(Bn, S, DIL_HEADS, DIL_HEAD_DIM), cos, sin)
    v = v.reshape(Bn, S, DIL_HEADS, DIL_HEAD_DIM)
    outs = []
    lses = []
    for g, (window, dil) in enumerate(DIL_PATTERNS):
        sl = slice(g * DIL_HEADS_PER_GROUP, (g + 1) * DIL_HEADS_PER_GROUP)
        o, lse = dilated_group_attention(q[:, :, sl], k[:, :, sl], v[:, :, sl], dil, window // dil)
        outs.append(o)
        lses.append(lse)
    alpha = jax.nn.softmax(jnp.stack(lses, axis=0), axis=0)
    o = jnp.stack(outs, axis=0) * alpha[..., None].astype(q.dtype)
    return o.transpose(1, 2, 0, 3, 4).reshape(Bn, S, DIL_DIM)


def causal_attention_blocks(q, k, v):
    Bn, S, H, dq = q.shape
    nb = S // ATTN_BLOCK
    scale = 1.0 / math.sqrt(dq)
    qb = q.reshape(Bn, nb, ATTN_BLOCK, H, dq).transpose(1, 0, 2, 3, 4)
    kpos = jnp.arange(S)

    def one_block(args):
        qblk, i = args
        s = jnp.einsum('bqhd,bkhd->bhqk', qblk, k).astype(jnp.float32) * scale
        qpos = i * ATTN_BLOCK + jnp.arange(ATTN_BLOCK)
        s = jnp.where(kpos[None, :] <= qpos[:, None], s, -jnp.inf)
        pr = jax.nn.softmax(s, axis=-1).astype(v.dtype)
        return jnp.einsum('bhqk,bkhd->bqhd', pr, v)

    o = lax.map(one_block, (qb, jnp.arange(nb)))
    return o.transpose(1, 0, 2, 3, 4).reshape(Bn, S, H, v.shape[-1])


def mla_mixer(cq, ckv, k_rope, q_norm_g, w_uq, kv_norm_g, w_ukv, cos, sin):
    Bn, S, _ = cq.shape
    q = (rmsnorm(cq, q_norm_g) @ w_uq).reshape(Bn, S, MLA_HEADS, MLA_QK)
    kv = (rmsnorm(ckv, kv_norm_g) @ w_ukv).reshape(Bn, S, MLA_HEADS, MLA_NOPE + MLA_V)
    q = jnp.concatenate([q[..., :MLA_NOPE], apply_rope(q[..., MLA_NOPE:], cos, sin)], axis=-1)
    kr = apply_rope(k_rope[:, :, None, :], cos, sin)
    k = jnp.concatenate([kv[..., :MLA_NOPE], jnp.broadcast_to(kr, (Bn, S, MLA_HEADS, MLA_ROPE))], axis=-1)
    v = kv[..., MLA_NOPE:]
    return causal_attention_blocks(q, k, v).reshape(Bn, S, MLA_DIM)


def hierarchical_moe(h, w_group, b_group, w_router, b_router, w_gate, w_up, w_down):
    T, D = h.shape
    g_logits = (h @ w_group).astype(jnp.float32) + b_group.astype(jnp.float32)
    g_prob = jax.nn.softmax(g_logits, axis=-1)
    g_top_p, g_top_i = lax.top_k(g_prob, 1)
    e_logits = ((h @ w_router).astype(jnp.float32) + b_router.astype(jnp.float32)).reshape(T, MOE_GROUPS, MOE_EPG)
    e_in = e_logits[jnp.arange(T), g_top_i[:, 0]]
    e_top_v, e_top_i = lax.top_k(e_in, MOE_TOPK)
    gates = jax.nn.softmax(e_top_v, axis=-1) * g_top_p
    expert_idx = g_top_i * MOE_EPG + e_top_i

    A = T * MOE_TOPK
    nblk = (A + N_EXPERTS * (MOE_BLOCK - 1) + MOE_BLOCK - 1) // MOE_BLOCK
    P = nblk * MOE_BLOCK
    flat_e = expert_idx.reshape(-1).astype(jnp.int32)
    order = jnp.argsort(flat_e)
    sorted_e = flat_e[order]
    tok = order // MOE_TOPK
    counts = jnp.bincount(flat_e, length=N_EXPERTS)
    padded = ((counts + MOE_BLOCK - 1) // MOE_BLOCK) * MOE_BLOCK
    pend = jnp.cumsum(padded)
    pstart = pend - padded
    start = jnp.cumsum(counts) - counts
    dest = pstart[sorted_e] + (jnp.arange(A) - start[sorted_e])
    buf_tok = jnp.zeros((P,), jnp.int32).at[dest].set(tok.astype(jnp.int32))
    block_expert = jnp.minimum(jnp.searchsorted(pend, jnp.arange(nblk) * MOE_BLOCK, side='right'), N_EXPERTS - 1)
    xb = h[buf_tok].reshape(nblk, MOE_BLOCK, D)

    def one_block(args):
        xblk, e = args
        return (jax.nn.silu(xblk @ w_gate[e]) * (xblk @ w_up[e])) @ w_down[e]

    yb = lax.map(one_block, (xb, block_expert)).reshape(P, D)
    contrib = yb[dest] * gates.reshape(-1)[order][:, None].astype(h.dtype)
    return jnp.zeros((T, D), h.dtype).at[tok].add(contrib)


def setup_inputs(seed: int = 0) -> dict:
    key = jax.random.key(seed)
    ks = iter(list(jax.random.split(key, 40)))
    f32 = jnp.float32

    def nrm(shape, scale):
        return jax.random.normal(next(ks), shape, f32) * scale

    def gain(shape):
        return 1.0 + 0.02 * jax.random.normal(next(ks), shape, f32)

    x = nrm((BATCH, SEQ, D_MODEL), 1.0)
    p = nrm((DEPTH, BATCH, SEQ, PLE_DIM), 1.0)
    positions = (jnp.arange(SEQ, dtype=jnp.int32)[None, :]
                 + jax.random.randint(next(ks), (BATCH, 1), 0, 1024, dtype=jnp.int32))
    norm_mix_g = gain((DEPTH, D_MODEL))
    w_in = nrm((DEPTH, D_MODEL, D_IN), D_MODEL ** -0.5)
    conv_w = nrm((DEPTH, CONV_K, CONV_DIM), CONV_K ** -0.5)
    ssm_lam_re = -0.5 * jnp.exp(nrm((DEPTH, SSM_GROUPS, SSM_STATE), 0.05))
    ssm_lam_im = math.pi * jnp.arange(SSM_STATE, dtype=f32) + nrm((DEPTH, SSM_GROUPS, SSM_STATE), 0.05)
    ssm_log_dt = jax.random.uniform(next(ks), (DEPTH, SSM_GROUPS), f32, math.log(1e-3), math.log(1e-1))
    ssm_b_re = nrm((DEPTH, SSM_GROUPS, SSM_STATE, SSM_GROUP), (2 * SSM_GROUP) ** -0.5)
    ssm_b_im = nrm((DEPTH, SSM_GROUPS, SSM_STATE, SSM_GROUP), (2 * SSM_GROUP) ** -0.5)
    ssm_c_re = nrm((DEPTH, SSM_GROUPS, SSM_GROUP, SSM_STATE), (2 * SSM_STATE) ** -0.5)
    ssm_c_im = nrm((DEPTH, SSM_GROUPS, SSM_GROUP, SSM_STATE), (2 * SSM_STATE) ** -0.5)
    ssm_d = nrm((DEPTH, SSM_DIM), 1.0)
    ssm_glu_w = nrm((DEPTH, SSM_DIM, SSM_DIM), SSM_DIM ** -0.5)
    ssm_glu_b = nrm((DEPTH, SSM_DIM), 0.02)
    mla_q_norm_g = gain((DEPTH, MLA_Q_RANK))
    mla_w_uq = nrm((DEPTH, MLA_Q_RANK, MLA_HEADS * MLA_QK), MLA_Q_RANK ** -0.5)
    mla_kv_norm_g = gain((DEPTH, MLA_KV_RANK))
    mla_w_ukv = nrm((DEPTH, MLA_KV_RANK, MLA_HEADS * (MLA_NOPE + MLA_V)), MLA_KV_RANK ** -0.5)
    w_out = nrm((DEPTH, D_MIX, D_MODEL), D_MIX ** -0.5)
    norm_ffn_g = gain((DEPTH, D_MODEL))
    w_group = nrm((DEPTH, D_MODEL, MOE_GROUPS), D_MODEL ** -0.5)
    b_group = nrm((DEPTH, MOE_GROUPS), 0.01)
    w_router = nrm((DEPTH, D_MODEL, N_EXPERTS), D_MODEL ** -0.5)
    b_router = nrm((DEPTH, N_EXPERTS), 0.01)
    moe_w_gate = nrm((DEPTH, N_EXPERTS, D_MODEL, MOE_FF), D_MODEL ** -0.5)
    moe_w_up = nrm((DEPTH, N_EXPERTS, D_MODEL, MOE_FF), D_MODEL ** -0.5)
    moe_w_down = nrm((DEPTH, N_EXPERTS, MOE_FF, D_MODEL), MOE_FF ** -0.5)
    norm_ple_g = gain((DEPTH, D_MODEL))
    ple_w_proj = nrm((DEPTH, PLE_DIM, D_MODEL), PLE_DIM ** -0.5)
    ple_w_gate = nrm((DEPTH, D_MODEL, D_MODEL), D_MODEL ** -0.5)
    final_norm_g = gain((D_MODEL,))
    return {'x': x, 'p': p, 'positions': positions, 'norm_mix_g': norm_mix_g, 'w_in': w_in,
            'conv_w': conv_w, 'ssm_lam_re': ssm_lam_re, 'ssm_lam_im': ssm_lam_im, 'ssm_log_dt': ssm_log_dt,
            'ssm_b_re': ssm_b_re, 'ssm_b_im': ssm_b_im, 'ssm_c_re': ssm_c_re, 'ssm_c_im': ssm_c_im,
            'ssm_d': ssm_d, 'ssm_glu_w': ssm_glu_w, 'ssm_glu_b': ssm_glu_b,
            'mla_q_norm_g': mla_q_norm_g, 'mla_w_uq': mla_w_uq, 'mla_kv_norm_g': mla_kv_norm_g,
            'mla_w_ukv': mla_w_ukv, 'w_out': w_out, 'norm_ffn_g': norm_ffn_g,
            'w_group': w_group, 'b_group': b_group, 'w_router': w_router, 'b_router': b_router,
            'moe_w_gate': moe_w_gate, 'moe_w_up': moe_w_up, 'moe_w_down': moe_w_down,
            'norm_ple_g': norm_ple_g, 'ple_w_proj': ple_w_proj, 'ple_w_gate': ple_w_gate,
            'final_norm_g': final_norm_g}


def reference(x, p, positions, norm_mix_g, w_in, conv_w, ssm_lam_re, ssm_lam_im, ssm_log_dt,
              ssm_b_re, ssm_b_im, ssm_c_re, ssm_c_im, ssm_d, ssm_glu_w, ssm_glu_b,
              mla_q_norm_g, mla_w_uq, mla_kv_norm_g, mla_w_ukv, w_out, norm_ffn_g,
              w_group, b_group, w_router, b_router, moe_w_gate, moe_w_up, moe_w_down,
              norm_ple_g, ple_w_proj, ple_w_gate, final_norm_g):
    Bn, S, D = x.shape
    cos_d, sin_d = rope_cos_sin(positions, DIL_ROT)
    cos_m, sin_m = rope_cos_sin(positions, MLA_ROPE)
    split_at = list(np.cumsum(IN_SPLITS)[:-1])
    h = x
    for i in range(DEPTH):
        hn = rmsnorm(h, norm_mix_g[i])
        z = hn @ w_in[i]
        cb, cc, ch, su, dq, dk, dv, mcq, mckv, mkr = jnp.split(z, split_at, axis=-1)
        y_conv = short_conv_mixer(cb, cc, ch, conv_w[i])
        y_ssm = s5_mixer(su, ssm_lam_re[i], ssm_lam_im[i], ssm_log_dt[i], ssm_b_re[i], ssm_b_im[i],
                         ssm_c_re[i], ssm_c_im[i], ssm_d[i], ssm_glu_w[i], ssm_glu_b[i])
        y_dil = dilated_mixer(dq, dk, dv, cos_d, sin_d)
        y_mla = mla_mixer(mcq, mckv, mkr, mla_q_norm_g[i], mla_w_uq[i], mla_kv_norm_g[i], mla_w_ukv[i], cos_m, sin_m)
        mix = jnp.concatenate([y_conv, y_ssm, y_dil, y_mla], axis=-1) @ w_out[i]
        h = h + mix
        hn = rmsnorm(h, norm_ffn_g[i])
        ffn = hierarchical_moe(hn.reshape(Bn * S, D), w_group[i], b_group[i], w_router[i], b_router[i],
                               moe_w_gate[i], moe_w_up[i], moe_w_down[i])
        h = h + ffn.reshape(Bn, S, D)
        hn = rmsnorm(h, norm_ple_g[i])
        h = h + (p[i].astype(h.dtype) @ ple_w_proj[i]) * jax.nn.sigmoid(hn @ ple_w_gate[i])
    return rmsnorm(h, final_norm_g)
```

```python
import numpy as np
import concourse.bass as bass
import concourse.mybir as mybir

F32 = mybir.dt.float32
BF16 = mybir.dt.bfloat16
I32 = mybir.dt.int32
ALU = mybir.AluOpType
AF = mybir.ActivationFunctionType
AX = mybir.AxisListType

ENGS = ("pe", "act", "dve", "pool", "sp")
NDMA = 24


class Dep:
    __slots__ = ("w", "r", "name")

    def __init__(self, name=""):
        self.w = None
        self.r = []
        self.name = name


class Prog:
    def __init__(self, nc):
        self.nc = nc
        self.ops = {e: [] for e in ENGS}
        self.cnt = {e: 0 for e in ENGS}
        self.waited = {e: {} for e in ENGS}
        self.dma_k = 0
        self.dma_cnt = [0] * NDMA
        self.stack = None

    def _need(self, eng, ev, waits):
        if ev is None:
            return
        key, val = ev
        if key == eng and eng == "pe":
            return
        cur = self.waited[eng].get(key, 0)
        if cur >= val:
            return
        if waits.get(key, 0) < val:
            waits[key] = val

    def _resolve(self, eng, reads, writes):
        waits = {}
        for d in reads:
            self._need(eng, d.w, waits)
        for d in writes:
            self._need(eng, d.w, waits)
            for ev in d.r:
                self._need(eng, ev, waits)
        for k, v in waits.items():
            self.waited[eng][k] = v
        return waits

    muted = False

    def op(self, eng, fn, reads=(), writes=()):
        if self.muted:
            return None
        waits = self._resolve(eng, reads, writes)
        self.cnt[eng] += 1
        ev = (eng, self.cnt[eng])
        self.ops[eng].append((fn, waits, ("c", eng)))
        for d in reads:
            d.r.append(ev)
        for d in writes:
            d.w = ev
            d.r = []
        return ev

    def dma(self, eng, fn, reads=(), writes=()):
        if self.muted:
            return None
        k = self.dma_k % NDMA
        self.dma_k += 1
        key = ("dma", k)
        waits = self._resolve(eng, reads, writes)
        if self.dma_cnt[k] > 0:
            prev = self.dma_cnt[k] * 16
            if self.waited[eng].get(key, 0) < prev:
                waits[key] = max(waits.get(key, 0), prev)
                self.waited[eng][key] = prev
        self.dma_cnt[k] += 1
        ev = (key, self.dma_cnt[k] * 16)
        self.ops[eng].append((fn, waits, ("d", k)))
        for d in reads:
            d.r.append(ev)
        for d in writes:
            d.w = ev
            d.r = []
        return ev

    def wait_all(self, eng, deps):
        waits = {}
        for d in deps:
            self._need(eng, d.w, waits)
        for k, v in waits.items():
            self.waited[eng][k] = v
        self.ops[eng].append((None, waits, None))

    def barrier(self):
        for e in ENGS:
            waits = {}
            for o in ENGS:
                if o != e and self.cnt[o] > self.waited[e].get(o, 0):
                    waits[o] = self.cnt[o]
            for k in range(NDMA):
                v = self.dma_cnt[k] * 16
                if v > self.waited[e].get(("dma", k), 0):
                    waits[("dma", k)] = v
            for k_, v in waits.items():
                self.waited[e][k_] = v
            self.ops[e].append((None, waits, None))

    def emit(self, stack):
        nc = self.nc
        if not hasattr(self, "sems"):
            self.sems = {}
            for e in ENGS:
                self.sems[e] = stack.enter_context(nc.semaphore("s_" + e))
            for k in range(NDMA):
                self.sems[("dma", k)] = stack.enter_context(nc.semaphore("s_dma%d" % k))
        sems = self.sems
        ops = self.ops
        self.ops = {e: [] for e in ENGS}
        with nc.Block() as block:
            def run(engname):
                def body(engine):
                    for fn, waits, inc in ops[engname]:
                        for key, val in waits.items():
                            engine.wait_ge(sems[key], val)
                        if fn is None:
                            continue
                        ins = fn(engine)
                        if inc[0] == "c":
                            ins.then_inc(sems[inc[1]], 1)
                        else:
                            ins.then_inc(sems[("dma", inc[1])], 16)
                return body

            block.tensor(run("pe"))
            block.scalar(run("act"))
            block.vector(run("dve"))
            block.gpsimd(run("pool"))
            block.sync(run("sp"))


from contextlib import ExitStack
from concourse.bass_utils import run_bass_kernel_spmd

D_MODEL = 2048
NTOK = 1024
MAGIC = 12582912.0
NORM_EPS = 1e-6

IN_SPLITS = (448, 448, 448, 448, 576, 576, 576, 384, 256, 32)
Z_PAD = (512, 512, 512, 512, 640, 640, 640, 384, 256, 128)
Z_OFF = [0]
for _p in Z_PAD:
    Z_OFF.append(Z_OFF[-1] + _p)
ZROWS = Z_OFF[-1]
ZCH = ZROWS // 128
MKR_SHIFT = 64


def z_colmap():
    cm = -np.ones(ZROWS, np.int64)
    src = 0
    for i, (n, _) in enumerate(zip(IN_SPLITS, Z_PAD)):
        off = Z_OFF[i] + (MKR_SHIFT if i == 9 else 0)
        cm[off:off + n] = np.arange(src, src + n)
        src += n
    return cm


class K:
    def __init__(self, nc, st):
        self.nc, self.st, self.P = nc, st, Prog(nc)
        self.n = 0

    def sb(self, shape, dt, name=None):
        self.n += 1
        t = self.st.enter_context(self.nc.sbuf_tensor(name or ("t%d" % self.n), list(shape), dt))
        return t, Dep()

    def ps(self, shape, dt=F32, name=None):
        self.n += 1
        t = self.st.enter_context(self.nc.psum_tensor(name or ("p%d" % self.n), list(shape), dt))
        return t, Dep()

    def dram_in(self, name, shape, dt=F32):
        return self.nc.dram_tensor(name, list(shape), dt, kind="ExternalInput").ap()

    def dram_out(self, name, shape, dt=F32):
        return self.nc.dram_tensor(name, list(shape), dt, kind="ExternalOutput").ap()

    def dma(self, q, out, in_, reads=(), writes=()):
        return self.P.dma(q, lambda e: e.dma_start(out=out, in_=in_), reads, writes)

    def mm(self, out, lhsT, rhs, start, stop, reads=(), writes=()):
        return self.P.op("pe", lambda e: e.matmul(out, lhsT, rhs, start=start, stop=stop), reads, writes)

    def act(self, out, in_, func, reads=(), writes=(), **kw):
        return self.P.op("act", lambda e: e.activation(out=out, in_=in_, func=func, **kw), reads, writes)

    def tt(self, eng, out, a, b, op, reads=(), writes=()):
        return self.P.op(eng, lambda e: e.tensor_tensor(out, a, b, op=op), reads, writes)

    def ts(self, eng, out, a, s1, s2, op0, op1=None, reads=(), writes=()):
        if op1 is None:
            return self.P.op(eng, lambda e: e.tensor_scalar(out, a, s1, None, op0=op0), reads, writes)
        return self.P.op(eng, lambda e: e.tensor_scalar(out, a, s1, s2, op0=op0, op1=op1), reads, writes)

    def stt(self, eng, out, a, s, b, op0, op1, reads=(), writes=()):
        return self.P.op(eng, lambda e: e.scalar_tensor_tensor(out, a, s, b, op0=op0, op1=op1), reads, writes)

    def copy(self, eng, out, in_, reads=(), writes=()):
        if eng == "act":
            return self.P.op("act", lambda e: e.activation(out=out, in_=in_, func=AF.Copy), reads, writes)
        return self.P.op(eng, lambda e: e.tensor_copy(out, in_), reads, writes)

    def memset(self, eng, ap, v, writes=()):
        return self.P.op(eng, lambda e: e.memset(ap, v), (), writes)


def rms_rstd(k, chunks, deps, ntok, nfeat, ones, d_ones, rstd, d_rstd, sq_bufs, ps_ss, d_ps):
    nch = len(chunks)
    for c, (ap, d) in enumerate(zip(chunks, deps)):
        rows = ap.shape[0]
        sq, d_sq = sq_bufs[c % len(sq_bufs)]
        k.act(sq[0:rows, 0:ntok], ap, AF.Square, reads=[d], writes=[d_sq])
        for t0 in range(0, ntok, 512):
            k.mm(ps_ss[:, t0:t0 + 512], ones[0:rows, :], sq[0:rows, t0:t0 + 512], start=(c == 0), stop=(c == nch - 1),
                 reads=[d_ones, d_sq], writes=[d_ps])
    k.ts("dve", rstd[:, 0:ntok], ps_ss[:, 0:ntok], 1.0 / nfeat, NORM_EPS, ALU.mult, ALU.add, reads=[d_ps], writes=[d_rstd])
    k.act(rstd[:, 0:ntok], rstd[:, 0:ntok], AF.Sqrt, reads=[d_rstd], writes=[d_rstd])
    k.P.op("dve", lambda e: e.reciprocal(rstd[:, 0:ntok], rstd[:, 0:ntok]), [d_rstd], [d_rstd])


def build_L1():
    nc = bass.Bass("TRN2", target_bir_lowering=False)
    with ExitStack() as st:
        k = K(nc, st)
        hT_d = k.dram_in("hT", [16, 128, NTOK])
        g_d = k.dram_in("g", [128, 16])
        w_d = k.dram_in("w", [ZCH, 128, 2048])
        z_d = k.dram_out("z", [ZCH, 128, NTOK])
        hT = [k.sb([128, NTOK], F32) for _ in range(16)]
        hn = [k.sb([128, NTOK], BF16) for _ in range(16)]
        g, d_g = k.sb([128, 16], F32)
        ones, d_ones = k.sb([128, 128], F32)
        rstd, d_rstd = k.sb([128, NTOK], F32)
        sqb = [k.sb([128, NTOK], F32) for _ in range(2)]
        wb = [k.sb([128, 2048], BF16) for _ in range(3)]
        zo = [k.sb([128, NTOK], F32) for _ in range(2)]
        ps_ss, d_ss = k.ps([128, NTOK])
        pz = [k.ps([128, NTOK]) for _ in range(2)]
        k.memset("pool", ones[:], 1.0, writes=[d_ones])
        k.dma("act", g[:], g_d, writes=[d_g])
        for c in range(16):
            k.dma("sp" if c % 2 == 0 else "act", hT[c][0][:], hT_d[c], writes=[hT[c][1]])
        rms_rstd(k, [hT[c][0][:] for c in range(16)], [hT[c][1] for c in range(16)], NTOK, D_MODEL,
                 ones, d_ones, rstd, d_rstd, sqb, ps_ss, d_ss)
        for c in range(16):
            k.stt("dve", hn[c][0][:], hT[c][0][:], g[:, c:c + 1], rstd[:],
                  ALU.mult, ALU.mult, reads=[hT[c][1], d_g, d_rstd], writes=[hn[c][1]])
        for m in range(ZCH):
            w, d_w = wb[m % 3]
            k.dma("pool", w[:], w_d[m], writes=[d_w])
            p, d_p = pz[m % 2]
            for t in range(NTOK // 512):
                for kc in range(16):
                    k.mm(p[:, t * 512:(t + 1) * 512], w[:, kc * 128:(kc + 1) * 128], hn[kc][0][:, t * 512:(t + 1) * 512],
                         start=(kc == 0), stop=(kc == 15), reads=[d_w, hn[kc][1]], writes=[d_p])
            o, d_o = zo[m % 2]
            k.copy("act", o[:, 0:512], p[:, 0:512], reads=[d_p], writes=[d_o])
            k.copy("dve", o[:, 512:1024], p[:, 512:1024], reads=[d_p], writes=[d_o])
            k.dma("sp", z_d[m], o[:], reads=[d_o], writes=[d_o])
        k.P.wait_all("sp", [zo[0][1], zo[1][1]])
        k.P.emit(st)
    return nc


def prep_L1_weights(w_in, g):
    cm = z_colmap()
    wp = np.zeros((2048, ZROWS), np.float32)
    valid = cm >= 0
    wp[:, valid] = w_in[:, cm[valid]]
    w = wp.reshape(16, 128, ZCH, 128).transpose(2, 1, 0, 3).reshape(ZCH, 128, 2048)
    gg = np.ascontiguousarray(g.reshape(16, 128).T)
    return np.ascontiguousarray(w), gg


MIXCH = 17
NEXP = 64
CAP = 128
FF = 512


def mix_rowmap():
    rm = -np.ones(MIXCH * 128, np.int64)
    rm[0:448] = np.arange(0, 448)
    rm[512:960] = np.arange(448, 896)
    rm[1024:1600] = np.arange(896, 1472)
    rm[1600:2176] = np.arange(1472, 2048)
    return rm


def build_L3():
    nc = bass.Bass("TRN2", target_bir_lowering=False)
    with ExitStack() as st:
        k = K(nc, st)
        P = k.P
        hT_d = k.dram_in("hT", [16, 128, NTOK])
        mix_d = k.dram_in("mix", [MIXCH, 128, NTOK])
        wout_d = k.dram_in("wout", [16, 128, MIXCH * 128])
        gffn_d = k.dram_in("gffn", [128, 16])
        wr_d = k.dram_in("wr", [128, 16 * 72])
        br_d = k.dram_in("br", [128, 72])
        ident_d = k.dram_in("ident", [128, 128])
        tri_d = k.dram_in("tri", [128, 128])
        ebase_d = k.dram_in("ebase", [128, 64])
        wg_d = k.dram_in("wg", [NEXP, 128, 16 * FF])
        wu_d = k.dram_in("wu", [NEXP, 128, 16 * FF])
        wd_d = k.dram_in("wd", [NEXP, 128, 4 * 2048])
        gple_d = k.dram_in("gple", [128, 16])
        pT_d = k.dram_in("pT", [2, 128, NTOK])
        wpp_d = k.dram_in("wpp", [16, 128, 2 * 128])
        wpg_d = k.dram_in("wpg", [16, 128, 16 * 128])
        gfin_d = k.dram_in("gfin", [128, 16])
        hout_d = k.dram_out("hout", [16, 128, NTOK])
        hnout_d = k.dram_out("hnout", [16, 128, NTOK])
        xbuf = nc.dram_tensor("xbuf", [NEXP * CAP, 2048], BF16).ap()
        ybuf = nc.dram_tensor("ybuf", [NEXP * CAP, 2048], BF16).ap()

        hT = [k.sb([128, NTOK], F32) for _ in range(16)]
        RW, _ = k.sb([128, 32768], BF16)
        F = [k.sb([128, NTOK], F32) for _ in range(3)]
        rstd, d_rstd = k.sb([128, NTOK], F32)
        xg = [k.sb([128, 2048], BF16) for _ in range(2)]
        xgT = [k.sb([128, 2048], BF16) for _ in range(2)]
        ye = [k.sb([128, 2048], BF16) for _ in range(2)]
        silu_t, d_silu = k.sb([128, FF], F32)
        actb, d_actb = k.sb([128, FF], BF16)
        actT, d_actT = k.sb([128, FF], BF16)
        gffn, d_gffn = k.sb([128, 16], F32)
        gple, d_gple = k.sb([128, 16], F32)
        gfin, d_gfin = k.sb([128, 16], F32)
        wr, d_wr = k.sb([128, 16 * 72], F32)
        br, d_br = k.sb([128, 72], F32)
        ident, d_ident = k.sb([128, 128], F32)
        identb, d_identb = k.sb([128, 128], BF16)
        tri, d_tri = k.sb([128, 128], F32)
        ebase, d_ebase = k.sb([128, 64], F32)
        ones, d_ones = k.sb([128, 128], F32)
        zero_b, d_zero = k.sb([128, 2048], BF16)
        A = [k.sb([128, 64], F32) for _ in range(8)]
        slot_f, d_slotf = k.sb([128, 16], F32)
        slot_i, d_sloti = k.sb([128, 16], I32)
        gates, d_gates = k.sb([128, 16], F32)
        lg, d_lg = k.sb([128, 72], F32)
        sm = [k.sb([128, 8], F32) for _ in range(6)]
        col = [k.sb([128, 1], F32) for _ in range(10)]
        oh = [k.sb([128, 64], F32) for _ in range(2)]
        tmp64, d_tmp64 = k.sb([128, 64], F32)

        pb = [k.ps([128, 512]) for _ in range(7)]
        ptb, d_ptb = k.ps([128, 1024], BF16)

        for c in range(16):
            k.dma("sp" if c % 2 == 0 else "act", hT[c][0][:], hT_d[c], writes=[hT[c][1]])
        k.dma("act", gffn[:], gffn_d, writes=[d_gffn])
        k.dma("act", gple[:], gple_d, writes=[d_gple])
        k.dma("act", gfin[:], gfin_d, writes=[d_gfin])
        k.dma("act", wr[:], wr_d, writes=[d_wr])
        k.dma("act", br[:], br_d, writes=[d_br])
        k.dma("act", ident[:], ident_d, writes=[d_ident])
        k.dma("pool", identb[:], ident_d, writes=[d_identb])
        k.dma("act", tri[:], tri_d, writes=[d_tri])
        k.dma("act", ebase[:], ebase_d, writes=[d_ebase])
        k.memset("pool", ones[:], 1.0, writes=[d_ones])
        k.memset("pool", zero_b[:], 0.0, writes=[d_zero])
        d_xbuf = Dep()
        for e in range(NEXP):
            k.dma("sp", xbuf[e * CAP:(e + 1) * CAP, :], zero_b[:], reads=[d_zero], writes=[d_xbuf])

        mixb = [RW[:, c * NTOK:(c + 1) * NTOK] for c in range(MIXCH)]
        d_mixb = [Dep() for _ in range(MIXCH)]
        WB0 = MIXCH * NTOK
        wob = [(RW[:, WB0 + i * MIXCH * 128: WB0 + (i + 1) * MIXCH * 128], Dep()) for i in range(3)]
        for c in range(MIXCH):
            k.dma("pool", mixb[c], mix_d[c], writes=[d_mixb[c]])
        for m in range(16):
            w, d_w = wob[m % 3]
            k.dma("pool", w, wout_d[m], writes=[d_w])
            for t in range(2):
                p, d_p = pb[(2 * m + t) % 4]
                for c in range(MIXCH):
                    k.mm(p[:, :], w[:, c * 128:(c + 1) * 128], mixb[c][:, t * 512:(t + 1) * 512],
                         start=(c == 0), stop=(c == MIXCH - 1), reads=[d_w, d_mixb[c]], writes=[d_p])
                k.tt("dve", hT[m][0][:, t * 512:(t + 1) * 512], hT[m][0][:, t * 512:(t + 1) * 512], p[:, :], ALU.add,
                     reads=[d_p, hT[m][1]], writes=[hT[m][1]])
        P.barrier()

        hn2 = [RW[:, c * NTOK:(c + 1) * NTOK] for c in range(16)]
        d_hn2 = [Dep() for _ in range(16)]
        ps_ss, d_ss = None, Dep()

        class _PS2:
            pass
        def rstd_feat(gtile, d_gt, outs, d_outs, out_dt_bf16=True):
            for c in range(16):
                sq, d_sq = F[c % 2]
                k.act(sq[:], hT[c][0][:], AF.Square, reads=[hT[c][1]], writes=[d_sq])
                for t in range(2):
                    k.mm(pb[t][0][:, :], ones[:], sq[:, t * 512:(t + 1) * 512], start=(c == 0), stop=(c == 15),
                         reads=[d_ones, d_sq], writes=[pb[t][1]])
            for t in range(2):
                k.ts("dve", rstd[:, t * 512:(t + 1) * 512], pb[t][0][:, :], 1.0 / D_MODEL, NORM_EPS, ALU.mult, ALU.add,
                     reads=[pb[t][1]], writes=[d_rstd])
            k.act(rstd[:], rstd[:], AF.Sqrt, reads=[d_rstd], writes=[d_rstd])
            P.op("dve", lambda e: e.reciprocal(rstd[:], rstd[:]), [d_rstd], [d_rstd])
            for c in range(16):
                k.stt("dve", outs[c], hT[c][0][:], gtile[:, c:c + 1], rstd[:], ALU.mult, ALU.mult,
                      reads=[hT[c][1], d_gt, d_rstd], writes=[d_outs[c]])

        rstd_feat(gffn, d_gffn, hn2, d_hn2)
        for c in range(16):
            k.ts("pool", wr[:, c * 72:(c + 1) * 72], wr[:, c * 72:(c + 1) * 72], gffn[:, c:c + 1], None, ALU.mult,
                 reads=[d_wr, d_gffn], writes=[d_wr])

        xtok = xg
        for tt in range(8):
            ts_ = slice(tt * 128, (tt + 1) * 128)
            pl, d_pl = pb[2 + (tt % 2)]
            pss, d_pss = pb[4 + (tt % 2)]
            for c in range(16):
                k.mm(pl[:, 0:72], hT[c][0][:, ts_], wr[:, c * 72:(c + 1) * 72], start=(c == 0), stop=(c == 15),
                     reads=[hT[c][1], d_wr], writes=[d_pl])
            for c in range(16):
                sq, d_sq = F[c % 2]
                k.act(sq[:, 0:128], hT[c][0][:, ts_], AF.Square, reads=[hT[c][1]], writes=[d_sq])
                k.mm(pss[:, 0:1], sq[:, 0:128], ones[:, 0:1], start=(c == 0), stop=(c == 15),
                     reads=[d_sq, d_ones], writes=[d_pss])
            rt, d_rt = col[0]
            k.ts("dve", rt[:], pss[:, 0:1], 1.0 / D_MODEL, NORM_EPS, ALU.mult, ALU.add, reads=[d_pss], writes=[d_rt])
            k.act(rt[:], rt[:], AF.Sqrt, reads=[d_rt], writes=[d_rt])
            P.op("dve", lambda e, rt=rt: e.reciprocal(rt[:], rt[:]), [d_rt], [d_rt])
            k.stt("dve", lg[:], pl[:, 0:72], rt[:, 0:1], br[:], ALU.mult, ALU.add, reads=[d_pl, d_rt, d_br], writes=[d_lg])
            gmax, d_gmax = col[1]
            P.op("dve", lambda e, gmax=gmax: e.reduce_max(gmax[:], lg[:, 0:8], axis=AX.X), [d_lg], [d_gmax])
            ngmax, d_ngmax = col[2]
            k.ts("dve", ngmax[:], gmax[:], -1.0, None, ALU.mult, reads=[d_gmax], writes=[d_ngmax])
            gexp, d_gexp = sm[0]
            gsum, d_gsum = col[3]
            k.act(gexp[:], lg[:, 0:8], AF.Exp, reads=[d_lg, d_ngmax], writes=[d_gexp, d_gsum], bias=ngmax[:, 0:1], accum_out=gsum[:, 0:1])
            gp, d_gp = col[4]
            P.op("dve", lambda e, gp=gp, gsum=gsum: e.reciprocal(gp[:], gsum[:]), [d_gsum], [d_gp])
            gmask, d_gmask = sm[1]
            k.ts("dve", gmask[:], lg[:, 0:8], gmax[:, 0:1], None, ALU.is_ge, reads=[d_lg, d_gmax], writes=[d_gmask])
            esel, d_esel = sm[2]
            k.ts("dve", esel[:], lg[:, 8:16], gmask[:, 0:1], None, ALU.mult, reads=[d_lg, d_gmask], writes=[d_esel])
            for g_ in range(1, 8):
                k.stt("dve", esel[:], lg[:, 8 + 8 * g_:16 + 8 * g_], gmask[:, g_:g_ + 1], esel[:], ALU.mult, ALU.add,
                      reads=[d_lg, d_gmask, d_esel], writes=[d_esel])
            v0, d_v0 = col[5]
            P.op("dve", lambda e, v0=v0, esel=esel: e.reduce_max(v0[:], esel[:], axis=AX.X), [d_esel], [d_v0])
            m0, d_m0 = sm[3]
            k.ts("dve", m0[:], esel[:], v0[:, 0:1], None, ALU.is_ge, reads=[d_esel, d_v0], writes=[d_m0])
            esel2, d_esel2 = sm[4]
            k.stt("dve", esel2[:], m0[:], -1e30, esel[:], ALU.mult, ALU.add, reads=[d_m0, d_esel], writes=[d_esel2])
            v1, d_v1 = col[6]
            P.op("dve", lambda e, v1=v1, esel2=esel2: e.reduce_max(v1[:], esel2[:], axis=AX.X), [d_esel2], [d_v1])
            m1, d_m1 = sm[5]
            k.ts("dve", m1[:], esel2[:], v1[:, 0:1], None, ALU.is_ge, reads=[d_esel2, d_v1], writes=[d_m1])
            dd, d_dd = col[7]
            k.tt("dve", dd[:], v1[:], v0[:], ALU.subtract, reads=[d_v1, d_v0], writes=[d_dd])
            ex, d_ex = col[8]
            k.act(ex[:], dd[:], AF.Exp, reads=[d_dd], writes=[d_ex])
            rr, d_rr = col[9]
            k.ts("dve", rr[:], ex[:], 1.0, None, ALU.add, reads=[d_ex], writes=[d_rr])
            P.op("dve", lambda e, rr=rr: e.reciprocal(rr[:], rr[:]), [d_rr], [d_rr])
            k.tt("dve", gates[:, 2 * tt:2 * tt + 1], rr[:], gp[:], ALU.mult, reads=[d_rr, d_gp, d_gates], writes=[d_gates])
            k.tt("dve", gates[:, 2 * tt + 1:2 * tt + 2], gates[:, 2 * tt:2 * tt + 1], ex[:], ALU.mult,
                 reads=[d_ex, d_gates], writes=[d_gates])
            for kk, (mk, d_mk) in enumerate(((m0, d_m0), (m1, d_m1))):
                for g_ in range(8):
                    k.ts("pool", oh[kk][0][:, 8 * g_:8 * g_ + 8], mk[:], gmask[:, g_:g_ + 1], None, ALU.mult,
                         reads=[d_mk, d_gmask, oh[kk][1]], writes=[oh[kk][1]])
            k.tt("dve", A[tt][0][:], oh[0][0][:], oh[1][0][:], ALU.add, reads=[oh[0][1], oh[1][1]], writes=[A[tt][1]])
            pc, d_pc = pb[6]
            for t2 in range(tt + 1):
                k.mm(pc[:, 0:64], (tri if t2 == tt else ones)[:], A[t2][0][:], start=(t2 == 0), stop=(t2 == tt),
                     reads=[d_tri, d_ones, A[t2][1]], writes=[d_pc])
            k.tt("dve", tmp64[:], pc[:, 0:64], ebase[:], ALU.add, reads=[d_pc, d_ebase], writes=[d_tmp64])
            for kk in range(2):
                k.tt("dve", oh[kk][0][:], oh[kk][0][:], tmp64[:], ALU.mult, reads=[d_tmp64, oh[kk][1]], writes=[oh[kk][1]])
                P.op("dve", lambda e, kk=kk, tt=tt: e.reduce_sum(slot_f[:, 2 * tt + kk:2 * tt + kk + 1], oh[kk][0][:], axis=AX.X),
                     [oh[kk][1], d_slotf], [d_slotf])
            k.copy("dve", slot_i[:, 2 * tt:2 * tt + 2], slot_f[:, 2 * tt:2 * tt + 2], reads=[d_slotf, d_sloti], writes=[d_sloti])
            xt, d_xt = xtok[tt % 2]
            for half in range(2):
                for c8 in range(8):
                    c = half * 8 + c8
                    P.op("pe", lambda e, c=c, c8=c8, ts_=ts_: e.transpose(ptb[:, c8 * 128:(c8 + 1) * 128], hn2[c][:, ts_], identb[:]),
                         [d_hn2[c], d_identb], [d_ptb])
                k.copy("act" if half == 0 else "dve", xt[:, half * 1024:(half + 1) * 1024], ptb[:, :], reads=[d_ptb], writes=[d_xt])
            for kk in range(2):
                P.dma("pool", lambda e, xt=xt, tt=tt, kk=kk: e.indirect_dma_start(
                    out=xbuf[:, :], out_offset=bass.IndirectOffsetOnAxis(ap=slot_i[:, 2 * tt + kk:2 * tt + kk + 1], axis=0),
                    in_=xt[:, :], in_offset=None), reads=[d_xt, d_sloti, d_xbuf], writes=[d_xt])
        P.barrier()

        wbuf = [(RW[:, i * 8192:(i + 1) * 8192], Dep()) for i in range(4)]
        wi = 0
        d_ybuf = Dep()
        for e_ in range(NEXP):
            x_, d_x = xg[e_ % 2]
            xT_, d_xT = xgT[e_ % 2]
            k.dma("sp", x_[:], xbuf[e_ * CAP:(e_ + 1) * CAP, :], writes=[d_x])
            wgt, d_wgt = wbuf[wi % 4]; wi += 1
            k.dma("pool", wgt, wg_d[e_], writes=[d_wgt])
            wut, d_wut = wbuf[wi % 4]; wi += 1
            k.dma("pool", wut, wu_d[e_], writes=[d_wut])
            wdt, d_wdt = wbuf[wi % 4]; wi += 1
            k.dma("pool", wdt, wd_d[e_], writes=[d_wdt])
            for half in range(2):
                for c8 in range(8):
                    c = half * 8 + c8
                    P.op("pe", lambda e, c=c, c8=c8, x_=x_: e.transpose(ptb[:, c8 * 128:(c8 + 1) * 128], x_[:, c * 128:(c + 1) * 128], identb[:]),
                         [d_x, d_identb], [d_ptb])
                k.copy("act" if half == 0 else "dve", xT_[:, half * 1024:(half + 1) * 1024], ptb[:, :], reads=[d_ptb], writes=[d_xT])
            pg, d_pg = pb[0]
            pu, d_pu = pb[1]
            for c in range(16):
                k.mm(pg[:, :], xT_[:, c * 128:(c + 1) * 128], wgt[:, c * FF:(c + 1) * FF], start=(c == 0), stop=(c == 15),
                     reads=[d_xT, d_wgt], writes=[d_pg])
            for c in range(16):
                k.mm(pu[:, :], xT_[:, c * 128:(c + 1) * 128], wut[:, c * FF:(c + 1) * FF], start=(c == 0), stop=(c == 15),
                     reads=[d_xT, d_wut], writes=[d_pu])
            k.act(silu_t[:], pg[:, :], AF.Silu, reads=[d_pg], writes=[d_silu])
            k.tt("dve", actb[:], silu_t[:], pu[:, :], ALU.mult, reads=[d_silu, d_pu], writes=[d_actb])
            for c4 in range(4):
                P.op("pe", lambda e, c4=c4: e.transpose(ptb[:, c4 * 128:(c4 + 1) * 128], actb[:, c4 * 128:(c4 + 1) * 128], identb[:]),
                     [d_actb, d_identb], [d_ptb])
            k.copy("act", actT[:], ptb[:, 0:512], reads=[d_ptb], writes=[d_actT])
            y_, d_y = ye[e_ % 2]
            for n in range(4):
                pd_, d_pd = pb[2 + n]
                for c4 in range(4):
                    k.mm(pd_[:, :], actT[:, c4 * 128:(c4 + 1) * 128], wdt[:, c4 * 2048 + n * 512: c4 * 2048 + (n + 1) * 512],
                         start=(c4 == 0), stop=(c4 == 3), reads=[d_actT, d_wdt], writes=[d_pd])
                k.copy("act" if n % 2 == 0 else "dve", y_[:, n * 512:(n + 1) * 512], pd_[:, :], reads=[d_pd], writes=[d_y])
            k.dma("sp", ybuf[e_ * CAP:(e_ + 1) * CAP, :], y_[:], reads=[d_y], writes=[d_y, d_ybuf])
        P.barrier()

        ytok = [(RW[:, i * 2048:(i + 1) * 2048], Dep()) for i in range(4)]
        ycomb = [(RW[:, 8192 + i * 2048: 8192 + (i + 1) * 2048], Dep()) for i in range(4)]
        ytmp, d_ytmp = F[2]
        for grp in range(2):
            for t4 in range(4):
                tt = grp * 4 + t4
                ys = []
                for kk in range(2):
                    yt, d_yt = ytok[(tt % 2) * 2 + kk]
                    P.dma("pool", lambda e, yt=yt, tt=tt, kk=kk: e.indirect_dma_start(
                        out=yt, out_offset=None, in_=ybuf[:, :],
                        in_offset=bass.IndirectOffsetOnAxis(ap=slot_i[:, 2 * tt + kk:2 * tt + kk + 1], axis=0)),
                        reads=[d_sloti, d_ybuf], writes=[d_yt])
                    ys.append((yt, d_yt))
                yc, d_yc = ycomb[t4]
                for h2 in range(2):
                    hs = slice(h2 * 1024, (h2 + 1) * 1024)
                    k.ts("dve", ytmp[:], ys[0][0][:, hs], gates[:, 2 * tt:2 * tt + 1], None, ALU.mult,
                         reads=[ys[0][1], d_gates], writes=[d_ytmp])
                    k.stt("dve", yc[:, hs], ys[1][0][:, hs], gates[:, 2 * tt + 1:2 * tt + 2], ytmp[:], ALU.mult, ALU.add,
                          reads=[ys[1][1], d_gates, d_ytmp], writes=[d_yc])
            for m in range(16):
                for t4 in range(4):
                    yc, d_yc = ycomb[t4]
                    P.op("pe", lambda e, m=m, t4=t4, yc=yc: e.transpose(ptb[:, t4 * 128:(t4 + 1) * 128], yc[:, m * 128:(m + 1) * 128], identb[:]),
                         [d_yc, d_identb], [d_ptb])
                k.tt("dve", hT[m][0][:, grp * 512:(grp + 1) * 512], hT[m][0][:, grp * 512:(grp + 1) * 512], ptb[:, 0:512], ALU.add,
                     reads=[d_ptb, hT[m][1]], writes=[hT[m][1]])
        P.barrier()

        hn3 = [RW[:, c * NTOK:(c + 1) * NTOK] for c in range(16)]
        d_hn3 = [Dep() for _ in range(16)]
        rstd_feat(gple, d_gple, hn3, d_hn3)
        G0 = 16 * NTOK
        pTb = [(RW[:, G0 + i * NTOK: G0 + (i + 1) * NTOK], Dep()) for i in range(2)]
        G1 = G0 + 2 * NTOK
        wpgb = [(RW[:, G1 + i * 2048: G1 + (i + 1) * 2048], Dep()) for i in range(3)]
        G2 = G1 + 3 * 2048
        wppb = [(RW[:, G2 + i * 256: G2 + (i + 1) * 256], Dep()) for i in range(3)]
        for c in range(2):
            k.dma("pool", pTb[c][0], pT_d[c], writes=[pTb[c][1]])
        sig, d_sig = F[2]
        for m in range(16):
            wg_, d_wg_ = wpgb[m % 3]
            wp_, d_wp_ = wppb[m % 3]
            k.dma("pool", wg_, wpg_d[m], writes=[d_wg_])
            k.dma("pool", wp_, wpp_d[m], writes=[d_wp_])
            for t in range(2):
                pgt, d_pgt = pb[(2 * m + t) % 4]
                ppj, d_ppj = pb[4 + (t % 2)]
                tsl = slice(t * 512, (t + 1) * 512)
                for c in range(16):
                    k.mm(pgt[:, :], wg_[:, c * 128:(c + 1) * 128], hn3[c][:, tsl], start=(c == 0), stop=(c == 15),
                         reads=[d_wg_, d_hn3[c]], writes=[d_pgt])
                for c in range(2):
                    k.mm(ppj[:, :], wp_[:, c * 128:(c + 1) * 128], pTb[c][0][:, tsl], start=(c == 0), stop=(c == 1),
                         reads=[d_wp_, pTb[c][1]], writes=[d_ppj])
                k.act(sig[:, tsl], pgt[:, :], AF.Sigmoid, reads=[d_pgt], writes=[d_sig])
                k.tt("dve", sig[:, tsl], sig[:, tsl], ppj[:, :], ALU.mult, reads=[d_ppj, d_sig], writes=[d_sig])
                k.tt("dve", hT[m][0][:, tsl], hT[m][0][:, tsl], sig[:, tsl], ALU.add, reads=[d_sig, hT[m][1]], writes=[hT[m][1]])
            k.dma("sp", hout_d[m], hT[m][0][:], reads=[hT[m][1]], writes=[])
        P.barrier()

        hno = [F[2][0], F[1][0]]
        d_hno = [F[2][1], F[1][1]]
        outs = [hno[c % 2] for c in range(16)]
        for c in range(16):
            sq, d_sq = F[0]
            k.act(sq[:], hT[c][0][:], AF.Square, reads=[hT[c][1]], writes=[d_sq])
            for t in range(2):
                k.mm(pb[t][0][:, :], ones[:], sq[:, t * 512:(t + 1) * 512], start=(c == 0), stop=(c == 15),
                     reads=[d_ones, d_sq], writes=[pb[t][1]])
        for t in range(2):
            k.ts("dve", rstd[:, t * 512:(t + 1) * 512], pb[t][0][:, :], 1.0 / D_MODEL, NORM_EPS, ALU.mult, ALU.add,
                 reads=[pb[t][1]], writes=[d_rstd])
        k.act(rstd[:], rstd[:], AF.Sqrt, reads=[d_rstd], writes=[d_rstd])
        P.op("dve", lambda e: e.reciprocal(rstd[:], rstd[:]), [d_rstd], [d_rstd])
        for c in range(16):
            o_, d_o = hno[c % 2], d_hno[c % 2]
            k.stt("dve", o_[:], hT[c][0][:], gfin[:, c:c + 1], rstd[:], ALU.mult, ALU.mult,
                  reads=[hT[c][1], d_gfin, d_rstd], writes=[d_o])
            k.dma("sp", hnout_d[c], o_[:], reads=[d_o], writes=[d_o])
        P.barrier()
        k.P.emit(st)
    return nc


def build_L3a():
    nc = bass.Bass("TRN2", target_bir_lowering=False)
    with ExitStack() as st:
        k = K(nc, st)
        P = k.P
        hT_d = k.dram_in("hT", [16, 128, NTOK])
        mix_d = k.dram_in("mix", [MIXCH, 128, NTOK])
        wout_d = k.dram_in("wout", [16, 128, MIXCH * 128])
        gffn_d = k.dram_in("gffn", [128, 16])
        wr_d = k.dram_in("wr", [128, 16 * 72])
        br_d = k.dram_in("br", [128, 72])
        ident_d = k.dram_in("ident", [128, 128])
        tri_d = k.dram_in("tri", [128, 128])
        ebase_d = k.dram_in("ebase", [128, 64])
        hout_d = k.dram_out("hout", [16, 128, NTOK])
        xbuf = k.dram_out("xbuf", [NEXP * CAP, 2048], BF16)
        sloto_d = k.dram_out("slot", [128, 16], I32)
        gateo_d = k.dram_out("gates", [128, 16])


        hT = [k.sb([128, NTOK], F32) for _ in range(16)]
        RW, _ = k.sb([128, 32768], BF16)
        F = [k.sb([128, NTOK], F32) for _ in range(3)]
        rstd, d_rstd = k.sb([128, NTOK], F32)
        xg = [k.sb([128, 2048], BF16) for _ in range(2)]
        xgT = [k.sb([128, 2048], BF16) for _ in range(2)]
        ye = [k.sb([128, 2048], BF16) for _ in range(2)]
        silu_t, d_silu = k.sb([128, FF], F32)
        actb, d_actb = k.sb([128, FF], BF16)
        actT, d_actT = k.sb([128, FF], BF16)
        gffn, d_gffn = k.sb([128, 16], F32)
        gple, d_gple = k.sb([128, 16], F32)
        gfin, d_gfin = k.sb([128, 16], F32)
        wr, d_wr = k.sb([128, 16 * 72], F32)
        br, d_br = k.sb([128, 72], F32)
        ident, d_ident = k.sb([128, 128], F32)
        identb, d_identb = k.sb([128, 128], BF16)
        tri, d_tri = k.sb([128, 128], F32)
        ebase, d_ebase = k.sb([128, 64], F32)
        ones, d_ones = k.sb([128, 128], F32)
        zero_b, d_zero = k.sb([128, 2048], BF16)
        A = [k.sb([128, 64], F32) for _ in range(8)]
        slot_f, d_slotf = k.sb([128, 16], F32)
        slot_i, d_sloti = k.sb([128, 16], I32)
        gates, d_gates = k.sb([128, 16], F32)
        lg, d_lg = k.sb([128, 72], F32)
        sm = [k.sb([128, 8], F32) for _ in range(6)]
        col = [k.sb([128, 1], F32) for _ in range(10)]
        oh = [k.sb([128, 64], F32) for _ in range(2)]
        tmp64, d_tmp64 = k.sb([128, 64], F32)

        pb = [k.ps([128, 512]) for _ in range(7)]
        ptb, d_ptb = k.ps([128, 1024], BF16)

        for c in range(16):
            k.dma("sp" if c % 2 == 0 else "act", hT[c][0][:], hT_d[c], writes=[hT[c][1]])
        k.dma("act", gffn[:], gffn_d, writes=[d_gffn])
        k.dma("act", wr[:], wr_d, writes=[d_wr])
        k.dma("act", br[:], br_d, writes=[d_br])
        k.dma("act", ident[:], ident_d, writes=[d_ident])
        k.dma("pool", identb[:], ident_d, writes=[d_identb])
        k.dma("act", tri[:], tri_d, writes=[d_tri])
        k.dma("act", ebase[:], ebase_d, writes=[d_ebase])
        k.memset("pool", ones[:], 1.0, writes=[d_ones])
        k.memset("pool", zero_b[:], 0.0, writes=[d_zero])
        d_xbuf = Dep()
        for e in range(NEXP):
            k.dma("sp", xbuf[e * CAP:(e + 1) * CAP, :], zero_b[:], reads=[d_zero], writes=[d_xbuf])


        mixb = [RW[:, c * NTOK:(c + 1) * NTOK] for c in range(MIXCH)]
        d_mixb = [Dep() for _ in range(MIXCH)]
        WB0 = MIXCH * NTOK
        wob = [(RW[:, WB0 + i * MIXCH * 128: WB0 + (i + 1) * MIXCH * 128], Dep()) for i in range(3)]
        for c in range(MIXCH):
            k.dma("pool", mixb[c], mix_d[c], writes=[d_mixb[c]])
        for m in range(16):
            w, d_w = wob[m % 3]
            k.dma("pool", w, wout_d[m], writes=[d_w])
            for t in range(2):
                p, d_p = pb[(2 * m + t) % 4]
                for c in range(MIXCH):
                    k.mm(p[:, :], w[:, c * 128:(c + 1) * 128], mixb[c][:, t * 512:(t + 1) * 512],
                         start=(c == 0), stop=(c == MIXCH - 1), reads=[d_w, d_mixb[c]], writes=[d_p])
                k.tt("dve", hT[m][0][:, t * 512:(t + 1) * 512], hT[m][0][:, t * 512:(t + 1) * 512], p[:, :], ALU.add,
                     reads=[d_p, hT[m][1]], writes=[hT[m][1]])
        P.barrier()

        hn2 = [RW[:, c * NTOK:(c + 1) * NTOK] for c in range(16)]
        d_hn2 = [Dep() for _ in range(16)]
        ps_ss, d_ss = None, Dep()

        class _PS2:
            pass
        def rstd_feat(gtile, d_gt, outs, d_outs, out_dt_bf16=True):
            for c in range(16):
                sq, d_sq = F[c % 2]
                k.act(sq[:], hT[c][0][:], AF.Square, reads=[hT[c][1]], writes=[d_sq])
                for t in range(2):
                    k.mm(pb[t][0][:, :], ones[:], sq[:, t * 512:(t + 1) * 512], start=(c == 0), stop=(c == 15),
                         reads=[d_ones, d_sq], writes=[pb[t][1]])
            for t in range(2):
                k.ts("dve", rstd[:, t * 512:(t + 1) * 512], pb[t][0][:, :], 1.0 / D_MODEL, NORM_EPS, ALU.mult, ALU.add,
                     reads=[pb[t][1]], writes=[d_rstd])
            k.act(rstd[:], rstd[:], AF.Sqrt, reads=[d_rstd], writes=[d_rstd])
            P.op("dve", lambda e: e.reciprocal(rstd[:], rstd[:]), [d_rstd], [d_rstd])
            for c in range(16):
                k.stt("dve", outs[c], hT[c][0][:], gtile[:, c:c + 1], rstd[:], ALU.mult, ALU.mult,
                      reads=[hT[c][1], d_gt, d_rstd], writes=[d_outs[c]])

        rstd_feat(gffn, d_gffn, hn2, d_hn2)
        for c in range(16):
            k.ts("pool", wr[:, c * 72:(c + 1) * 72], wr[:, c * 72:(c + 1) * 72], gffn[:, c:c + 1], None, ALU.mult,
                 reads=[d_wr, d_gffn], writes=[d_wr])

        xtok = xg
        for tt in range(8):
            ts_ = slice(tt * 128, (tt + 1) * 128)
            pl, d_pl = pb[2 + (tt % 2)]
            pss, d_pss = pb[4 + (tt % 2)]
            for c in range(16):
                k.mm(pl[:, 0:72], hT[c][0][:, ts_], wr[:, c * 72:(c + 1) * 72], start=(c == 0), stop=(c == 15),
                     reads=[hT[c][1], d_wr], writes=[d_pl])
            for c in range(16):
                sq, d_sq = F[c % 2]
                k.act(sq[:, 0:128], hT[c][0][:, ts_], AF.Square, reads=[hT[c][1]], writes=[d_sq])
                k.mm(pss[:, 0:1], sq[:, 0:128], ones[:, 0:1], start=(c == 0), stop=(c == 15),
                     reads=[d_sq, d_ones], writes=[d_pss])
            rt, d_rt = col[0]
            k.ts("dve", rt[:], pss[:, 0:1], 1.0 / D_MODEL, NORM_EPS, ALU.mult, ALU.add, reads=[d_pss], writes=[d_rt])
            k.act(rt[:], rt[:], AF.Sqrt, reads=[d_rt], writes=[d_rt])
            P.op("dve", lambda e, rt=rt: e.reciprocal(rt[:], rt[:]), [d_rt], [d_rt])
            k.stt("dve", lg[:], pl[:, 0:72], rt[:, 0:1], br[:], ALU.mult, ALU.add, reads=[d_pl, d_rt, d_br], writes=[d_lg])
            gmax, d_gmax = col[1]
            P.op("dve", lambda e, gmax=gmax: e.reduce_max(gmax[:], lg[:, 0:8], axis=AX.X), [d_lg], [d_gmax])
            ngmax, d_ngmax = col[2]
            k.ts("dve", ngmax[:], gmax[:], -1.0, None, ALU.mult, reads=[d_gmax], writes=[d_ngmax])
            gexp, d_gexp = sm[0]
            gsum, d_gsum = col[3]
            k.act(gexp[:], lg[:, 0:8], AF.Exp, reads=[d_lg, d_ngmax], writes=[d_gexp, d_gsum], bias=ngmax[:, 0:1], accum_out=gsum[:, 0:1])
            gp, d_gp = col[4]
            P.op("dve", lambda e, gp=gp, gsum=gsum: e.reciprocal(gp[:], gsum[:]), [d_gsum], [d_gp])
            gmask, d_gmask = sm[1]
            k.ts("dve", gmask[:], lg[:, 0:8], gmax[:, 0:1], None, ALU.is_ge, reads=[d_lg, d_gmax], writes=[d_gmask])
            esel, d_esel = sm[2]
            k.ts("dve", esel[:], lg[:, 8:16], gmask[:, 0:1], None, ALU.mult, reads=[d_lg, d_gmask], writes=[d_esel])
            for g_ in range(1, 8):
                k.stt("dve", esel[:], lg[:, 8 + 8 * g_:16 + 8 * g_], gmask[:, g_:g_ + 1], esel[:], ALU.mult, ALU.add,
                      reads=[d_lg, d_gmask, d_esel], writes=[d_esel])
            v0, d_v0 = col[5]
            P.op("dve", lambda e, v0=v0, esel=esel: e.reduce_max(v0[:], esel[:], axis=AX.X), [d_esel], [d_v0])
            m0, d_m0 = sm[3]
            k.ts("dve", m0[:], esel[:], v0[:, 0:1], None, ALU.is_ge, reads=[d_esel, d_v0], writes=[d_m0])
            esel2, d_esel2 = sm[4]
            k.stt("dve", esel2[:], m0[:], -1e30, esel[:], ALU.mult, ALU.add, reads=[d_m0, d_esel], writes=[d_esel2])
            v1, d_v1 = col[6]
            P.op("dve", lambda e, v1=v1, esel2=esel2: e.reduce_max(v1[:], esel2[:], axis=AX.X), [d_esel2], [d_v1])
            m1, d_m1 = sm[5]
            k.ts("dve", m1[:], esel2[:], v1[:, 0:1], None, ALU.is_ge, reads=[d_esel2, d_v1], writes=[d_m1])
            dd, d_dd = col[7]
            k.tt("dve", dd[:], v1[:], v0[:], ALU.subtract, reads=[d_v1, d_v0], writes=[d_dd])
            ex, d_ex = col[8]
            k.act(ex[:], dd[:], AF.Exp, reads=[d_dd], writes=[d_ex])
            rr, d_rr = col[9]
            k.ts("dve", rr[:], ex[:], 1.0, None, ALU.add, reads=[d_ex], writes=[d_rr])
            P.op("dve", lambda e, rr=rr: e.reciprocal(rr[:], rr[:]), [d_rr], [d_rr])
            k.tt("dve", gates[:, 2 * tt:2 * tt + 1], rr[:], gp[:], ALU.mult, reads=[d_rr, d_gp, d_gates], writes=[d_gates])
            k.tt("dve", gates[:, 2 * tt + 1:2 * tt + 2], gates[:, 2 * tt:2 * tt + 1], ex[:], ALU.mult,
                 reads=[d_ex, d_gates], writes=[d_gates])
            for kk, (mk, d_mk) in enumerate(((m0, d_m0), (m1, d_m1))):
                for g_ in range(8):
                    k.ts("pool", oh[kk][0][:, 8 * g_:8 * g_ + 8], mk[:], gmask[:, g_:g_ + 1], None, ALU.mult,
                         reads=[d_mk, d_gmask, oh[kk][1]], writes=[oh[kk][1]])
            k.tt("dve", A[tt][0][:], oh[0][0][:], oh[1][0][:], ALU.add, reads=[oh[0][1], oh[1][1]], writes=[A[tt][1]])
            pc, d_pc = pb[6]
            for t2 in range(tt + 1):
                k.mm(pc[:, 0:64], (tri if t2 == tt else ones)[:], A[t2][0][:], start=(t2 == 0), stop=(t2 == tt),
                     reads=[d_tri, d_ones, A[t2][1]], writes=[d_pc])
            k.tt("dve", tmp64[:], pc[:, 0:64], ebase[:], ALU.add, reads=[d_pc, d_ebase], writes=[d_tmp64])
            for kk in range(2):
                k.tt("dve", oh[kk][0][:], oh[kk][0][:], tmp64[:], ALU.mult, reads=[d_tmp64, oh[kk][1]], writes=[oh[kk][1]])
                P.op("dve", lambda e, kk=kk, tt=tt: e.reduce_sum(slot_f[:, 2 * tt + kk:2 * tt + kk + 1], oh[kk][0][:], axis=AX.X),
                     [oh[kk][1], d_slotf], [d_slotf])
            k.copy("dve", slot_i[:, 2 * tt:2 * tt + 2], slot_f[:, 2 * tt:2 * tt + 2], reads=[d_slotf, d_sloti], writes=[d_sloti])
            xt, d_xt = xtok[tt % 2]
            for half in range(2):
                for c8 in range(8):
                    c = half * 8 + c8
                    P.op("pe", lambda e, c=c, c8=c8, ts_=ts_: e.transpose(ptb[:, c8 * 128:(c8 + 1) * 128], hn2[c][:, ts_], identb[:]),
                         [d_hn2[c], d_identb], [d_ptb])
                k.copy("act" if half == 0 else "dve", xt[:, half * 1024:(half + 1) * 1024], ptb[:, :], reads=[d_ptb], writes=[d_xt])
            for kk in range(2):
                P.dma("pool", lambda e, xt=xt, tt=tt, kk=kk: e.indirect_dma_start(
                    out=xbuf[:, :], out_offset=bass.IndirectOffsetOnAxis(ap=slot_i[:, 2 * tt + kk:2 * tt + kk + 1], axis=0),
                    in_=xt[:, :], in_offset=None), reads=[d_xt, d_sloti, d_xbuf], writes=[d_xt])
        P.barrier()

        for c in range(16):
            k.dma("sp", hout_d[c], hT[c][0][:], reads=[hT[c][1]], writes=[])
        k.dma("sp", sloto_d, slot_i[:], reads=[d_sloti], writes=[])
        k.dma("sp", gateo_d, gates[:], reads=[d_gates], writes=[])
        P.barrier()
        k.P.emit(st)
    return nc


def build_L3b():
    nc = bass.Bass("TRN2", target_bir_lowering=False)
    with ExitStack() as st:
        k = K(nc, st)
        P = k.P
        xin = k.dram_in("xin", [NEXP * CAP, 2048], BF16)
        ident_d = k.dram_in("ident", [128, 128])
        wg_d = k.dram_in("wg", [8, 2048, FF])
        wu_d = k.dram_in("wu", [8, 2048, FF])
        wd_d = k.dram_in("wd", [8, FF, 2048])
        yout = k.dram_out("yout", [NEXP * CAP, 2048], BF16)
        xg = [k.sb([128, 2048], BF16) for _ in range(2)]
        xgT = [k.sb([128, 2048], BF16) for _ in range(2)]
        ye = [k.sb([128, 2048], BF16) for _ in range(2)]
        silu_t, d_silu = k.sb([128, FF], F32)
        actb, d_actb = k.sb([128, FF], BF16)
        actT, d_actT = k.sb([128, FF], BF16)
        identb, d_identb = k.sb([128, 128], BF16)
        pb = [k.ps([128, 512]) for _ in range(7)]
        ptb, d_ptb = k.ps([128, 1024], BF16)
        wbuf = [k.sb([128, 8192], BF16) for _ in range(6)]
        k.dma("pool", identb[:], ident_d, writes=[d_identb])
        d_ybuf = Dep()
        it = 0
        for j in range(8):
            wgt, d_wgt = wbuf[(j % 2) * 3 + 0]
            wut, d_wut = wbuf[(j % 2) * 3 + 1]
            wdt, d_wdt = wbuf[(j % 2) * 3 + 2]
            k.dma("pool", wgt[:].rearrange("p (c f) -> p c f", f=FF), wg_d[j].rearrange("(c p) f -> p c f", p=128), writes=[d_wgt])
            k.dma("pool", wut[:].rearrange("p (c f) -> p c f", f=FF), wu_d[j].rearrange("(c p) f -> p c f", p=128), writes=[d_wut])
            k.dma("pool", wdt[:].rearrange("p (c f) -> p c f", f=2048), wd_d[j].rearrange("(c p) f -> p c f", p=128), writes=[d_wdt])
            for s_ in range(8):
                r0 = (s_ * 8 + j) * CAP
                x_, d_x = xg[it % 2]
                xT_, d_xT = xgT[it % 2]
                y_, d_y = ye[it % 2]
                it += 1
                k.dma("sp", x_[:], xin[r0:r0 + CAP, :], writes=[d_x])
                for half in range(2):
                    for c8 in range(8):
                        c = half * 8 + c8
                        P.op("pe", lambda e, c=c, c8=c8, x_=x_: e.transpose(ptb[:, c8 * 128:(c8 + 1) * 128], x_[:, c * 128:(c + 1) * 128], identb[:]),
                             [d_x, d_identb], [d_ptb])
                    k.copy("act" if half == 0 else "dve", xT_[:, half * 1024:(half + 1) * 1024], ptb[:, :], reads=[d_ptb], writes=[d_xT])
                pg, d_pg = pb[0]
                pu, d_pu = pb[1]
                for c in range(16):
                    k.mm(pg[:, :], xT_[:, c * 128:(c + 1) * 128], wgt[:, c * FF:(c + 1) * FF], start=(c == 0), stop=(c == 15),
                         reads=[d_xT, d_wgt], writes=[d_pg])
                for c in range(16):
                    k.mm(pu[:, :], xT_[:, c * 128:(c + 1) * 128], wut[:, c * FF:(c + 1) * FF], start=(c == 0), stop=(c == 15),
                         reads=[d_xT, d_wut], writes=[d_pu])
                k.act(silu_t[:], pg[:, :], AF.Silu, reads=[d_pg], writes=[d_silu])
                k.tt("dve", actb[:], silu_t[:], pu[:, :], ALU.mult, reads=[d_silu, d_pu], writes=[d_actb])
                for c4 in range(4):
                    P.op("pe", lambda e, c4=c4: e.transpose(ptb[:, c4 * 128:(c4 + 1) * 128], actb[:, c4 * 128:(c4 + 1) * 128], identb[:]),
                         [d_actb, d_identb], [d_ptb])
                k.copy("act", actT[:], ptb[:, 0:512], reads=[d_ptb], writes=[d_actT])
                for n in range(4):
                    pd_, d_pd = pb[2 + n]
                    for c4 in range(4):
                        k.mm(pd_[:, :], actT[:, c4 * 128:(c4 + 1) * 128], wdt[:, c4 * 2048 + n * 512: c4 * 2048 + (n + 1) * 512],
                             start=(c4 == 0), stop=(c4 == 3), reads=[d_actT, d_wdt], writes=[d_pd])
                    k.copy("act" if n % 2 == 0 else "dve", y_[:, n * 512:(n + 1) * 512], pd_[:, :], reads=[d_pd], writes=[d_y])
                k.dma("sp", yout[r0:r0 + CAP, :], y_[:], reads=[d_y], writes=[d_y, d_ybuf])
        P.barrier()
        P.emit(st)
    return nc


def build_L3c():
    nc = bass.Bass("TRN2", target_bir_lowering=False)
    with ExitStack() as st:
        k = K(nc, st)
        P = k.P
        hT_d = k.dram_in("hT", [16, 128, NTOK])
        ident_d = k.dram_in("ident", [128, 128])
        gple_d = k.dram_in("gple", [128, 16])
        pT_d = k.dram_in("pT", [2, 128, NTOK])
        wpp_d = k.dram_in("wpp", [16, 128, 2 * 128])
        wpg_d = k.dram_in("wpg", [16, 128, 16 * 128])
        gfin_d = k.dram_in("gfin", [128, 16])
        hout_d = k.dram_out("hout", [16, 128, NTOK])
        hnout_d = k.dram_out("hnout", [16, 128, NTOK])
        ybuf = k.dram_in("ybuf", [NEXP * CAP, 2048], BF16)
        sloti_d = k.dram_in("slot", [128, 16], I32)
        gatei_d = k.dram_in("gates", [128, 16])


        hT = [k.sb([128, NTOK], F32) for _ in range(16)]
        RW, _ = k.sb([128, 32768], BF16)
        F = [k.sb([128, NTOK], F32) for _ in range(3)]
        rstd, d_rstd = k.sb([128, NTOK], F32)
        xg = [k.sb([128, 2048], BF16) for _ in range(2)]
        xgT = [k.sb([128, 2048], BF16) for _ in range(2)]
        ye = [k.sb([128, 2048], BF16) for _ in range(2)]
        silu_t, d_silu = k.sb([128, FF], F32)
        actb, d_actb = k.sb([128, FF], BF16)
        actT, d_actT = k.sb([128, FF], BF16)
        gffn, d_gffn = k.sb([128, 16], F32)
        gple, d_gple = k.sb([128, 16], F32)
        gfin, d_gfin = k.sb([128, 16], F32)
        wr, d_wr = k.sb([128, 16 * 72], F32)
        br, d_br = k.sb([128, 72], F32)
        ident, d_ident = k.sb([128, 128], F32)
        identb, d_identb = k.sb([128, 128], BF16)
        tri, d_tri = k.sb([128, 128], F32)
        ebase, d_ebase = k.sb([128, 64], F32)
        ones, d_ones = k.sb([128, 128], F32)
        zero_b, d_zero = k.sb([128, 2048], BF16)
        A = [k.sb([128, 64], F32) for _ in range(8)]
        slot_f, d_slotf = k.sb([128, 16], F32)
        slot_i, d_sloti = k.sb([128, 16], I32)
        gates, d_gates = k.sb([128, 16], F32)
        lg, d_lg = k.sb([128, 72], F32)
        sm = [k.sb([128, 8], F32) for _ in range(6)]
        col = [k.sb([128, 1], F32) for _ in range(10)]
        oh = [k.sb([128, 64], F32) for _ in range(2)]
        tmp64, d_tmp64 = k.sb([128, 64], F32)

        pb = [k.ps([128, 512]) for _ in range(7)]
        ptb, d_ptb = k.ps([128, 1024], BF16)

        for c in range(16):
            k.dma("sp" if c % 2 == 0 else "act", hT[c][0][:], hT_d[c], writes=[hT[c][1]])
        k.dma("act", gple[:], gple_d, writes=[d_gple])
        k.dma("act", gfin[:], gfin_d, writes=[d_gfin])
        k.dma("pool", identb[:], ident_d, writes=[d_identb])
        k.dma("act", slot_i[:], sloti_d, writes=[d_sloti])
        k.dma("act", gates[:], gatei_d, writes=[d_gates])
        k.memset("pool", ones[:], 1.0, writes=[d_ones])
        d_ybuf = Dep()
        def rstd_feat(gtile, d_gt, outs, d_outs, out_dt_bf16=True):
            for c in range(16):
                sq, d_sq = F[c % 2]
                k.act(sq[:], hT[c][0][:], AF.Square, reads=[hT[c][1]], writes=[d_sq])
                for t in range(2):
                    k.mm(pb[t][0][:, :], ones[:], sq[:, t * 512:(t + 1) * 512], start=(c == 0), stop=(c == 15),
                         reads=[d_ones, d_sq], writes=[pb[t][1]])
            for t in range(2):
                k.ts("dve", rstd[:, t * 512:(t + 1) * 512], pb[t][0][:, :], 1.0 / D_MODEL, NORM_EPS, ALU.mult, ALU.add,
                     reads=[pb[t][1]], writes=[d_rstd])
            k.act(rstd[:], rstd[:], AF.Sqrt, reads=[d_rstd], writes=[d_rstd])
            P.op("dve", lambda e: e.reciprocal(rstd[:], rstd[:]), [d_rstd], [d_rstd])
            for c in range(16):
                k.stt("dve", outs[c], hT[c][0][:], gtile[:, c:c + 1], rstd[:], ALU.mult, ALU.mult,
                      reads=[hT[c][1], d_gt, d_rstd], writes=[d_outs[c]])

        ytok = [(RW[:, i * 2048:(i + 1) * 2048], Dep()) for i in range(4)]
        ycomb = [(RW[:, 8192 + i * 2048: 8192 + (i + 1) * 2048], Dep()) for i in range(4)]
        ytmp, d_ytmp = F[2]
        for grp in range(2):
            for t4 in range(4):
                tt = grp * 4 + t4
                ys = []
                for kk in range(2):
                    yt, d_yt = ytok[(tt % 2) * 2 + kk]
                    P.dma("pool", lambda e, yt=yt, tt=tt, kk=kk: e.indirect_dma_start(
                        out=yt, out_offset=None, in_=ybuf[:, :],
                        in_offset=bass.IndirectOffsetOnAxis(ap=slot_i[:, 2 * tt + kk:2 * tt + kk + 1], axis=0)),
                        reads=[d_sloti, d_ybuf], writes=[d_yt])
                    ys.append((yt, d_yt))
                yc, d_yc = ycomb[t4]
                for h2 in range(2):
                    hs = slice(h2 * 1024, (h2 + 1) * 1024)
                    k.ts("dve", ytmp[:], ys[0][0][:, hs], gates[:, 2 * tt:2 * tt + 1], None, ALU.mult,
                         reads=[ys[0][1], d_gates], writes=[d_ytmp])
                    k.stt("dve", yc[:, hs], ys[1][0][:, hs], gates[:, 2 * tt + 1:2 * tt + 2], ytmp[:], ALU.mult, ALU.add,
                          reads=[ys[1][1], d_gates, d_ytmp], writes=[d_yc])
            for m in range(16):
                for t4 in range(4):
                    yc, d_yc = ycomb[t4]
                    P.op("pe", lambda e, m=m, t4=t4, yc=yc: e.transpose(ptb[:, t4 * 128:(t4 + 1) * 128], yc[:, m * 128:(m + 1) * 128], identb[:]),
                         [d_yc, d_identb], [d_ptb])
                k.tt("dve", hT[m][0][:, grp * 512:(grp + 1) * 512], hT[m][0][:, grp * 512:(grp + 1) * 512], ptb[:, 0:512], ALU.add,
                     reads=[d_ptb, hT[m][1]], writes=[hT[m][1]])
        P.barrier()

        hn3 = [RW[:, c * NTOK:(c + 1) * NTOK] for c in range(16)]
        d_hn3 = [Dep() for _ in range(16)]
        rstd_feat(gple, d_gple, hn3, d_hn3)
        G0 = 16 * NTOK
        pTb = [(RW[:, G0 + i * NTOK: G0 + (i + 1) * NTOK], Dep()) for i in range(2)]
        G1 = G0 + 2 * NTOK
        wpgb = [(RW[:, G1 + i * 2048: G1 + (i + 1) * 2048], Dep()) for i in range(3)]
        G2 = G1 + 3 * 2048
        wppb = [(RW[:, G2 + i * 256: G2 + (i + 1) * 256], Dep()) for i in range(3)]
        for c in range(2):
            k.dma("pool", pTb[c][0], pT_d[c], writes=[pTb[c][1]])
        sig, d_sig = F[2]
        for m in range(16):
            wg_, d_wg_ = wpgb[m % 3]
            wp_, d_wp_ = wppb[m % 3]
            k.dma("pool", wg_, wpg_d[m], writes=[d_wg_])
            k.dma("pool", wp_, wpp_d[m], writes=[d_wp_])
            for t in range(2):
                pgt, d_pgt = pb[(2 * m + t) % 4]
                ppj, d_ppj = pb[4 + (t % 2)]
                tsl = slice(t * 512, (t + 1) * 512)
                for c in range(16):
                    k.mm(pgt[:, :], wg_[:, c * 128:(c + 1) * 128], hn3[c][:, tsl], start=(c == 0), stop=(c == 15),
                         reads=[d_wg_, d_hn3[c]], writes=[d_pgt])
                for c in range(2):
                    k.mm(ppj[:, :], wp_[:, c * 128:(c + 1) * 128], pTb[c][0][:, tsl], start=(c == 0), stop=(c == 1),
                         reads=[d_wp_, pTb[c][1]], writes=[d_ppj])
                k.act(sig[:, tsl], pgt[:, :], AF.Sigmoid, reads=[d_pgt], writes=[d_sig])
                k.tt("dve", sig[:, tsl], sig[:, tsl], ppj[:, :], ALU.mult, reads=[d_ppj, d_sig], writes=[d_sig])
                k.tt("dve", hT[m][0][:, tsl], hT[m][0][:, tsl], sig[:, tsl], ALU.add, reads=[d_sig, hT[m][1]], writes=[hT[m][1]])
            k.dma("sp", hout_d[m], hT[m][0][:], reads=[hT[m][1]], writes=[])
        P.barrier()

        hno = [F[2][0], F[1][0]]
        d_hno = [F[2][1], F[1][1]]
        outs = [hno[c % 2] for c in range(16)]
        for c in range(16):
            sq, d_sq = F[0]
            k.act(sq[:], hT[c][0][:], AF.Square, reads=[hT[c][1]], writes=[d_sq])
            for t in range(2):
                k.mm(pb[t][0][:, :], ones[:], sq[:, t * 512:(t + 1) * 512], start=(c == 0), stop=(c == 15),
                     reads=[d_ones, d_sq], writes=[pb[t][1]])
        for t in range(2):
            k.ts("dve", rstd[:, t * 512:(t + 1) * 512], pb[t][0][:, :], 1.0 / D_MODEL, NORM_EPS, ALU.mult, ALU.add,
                 reads=[pb[t][1]], writes=[d_rstd])
        k.act(rstd[:], rstd[:], AF.Sqrt, reads=[d_rstd], writes=[d_rstd])
        P.op("dve", lambda e: e.reciprocal(rstd[:], rstd[:]), [d_rstd], [d_rstd])
        for c in range(16):
            o_, d_o = hno[c % 2], d_hno[c % 2]
            k.stt("dve", o_[:], hT[c][0][:], gfin[:, c:c + 1], rstd[:], ALU.mult, ALU.mult,
                  reads=[hT[c][1], d_gfin, d_rstd], writes=[d_o])
            k.dma("sp", hnout_d[c], o_[:], reads=[d_o], writes=[d_o])
        P.barrier()
        k.P.emit(st)
    return nc


def chunk_major(w, nk):
    ncol = w.shape[1]
    return np.ascontiguousarray(w.reshape(nk, 128, ncol).transpose(1, 0, 2).reshape(128, nk * ncol))


def out_chunk_major(w, nk):
    nm = w.shape[1] // 128
    return np.ascontiguousarray(w.reshape(nk, 128, nm, 128).transpose(2, 1, 0, 3).reshape(nm, 128, nk * 128))


def gvec(g):
    return np.ascontiguousarray(g.reshape(-1, 128).T)


def prep_L3_weights(inp, i):
    rm = mix_rowmap()
    valid = rm >= 0
    wo = np.zeros((MIXCH * 128, 2048), np.float32)
    wo[valid] = inp["w_out"][i][rm[valid]]
    d = {}
    d["wout"] = out_chunk_major(wo, MIXCH)
    d["gffn"] = gvec(inp["norm_ffn_g"][i])
    d["wr"] = chunk_major(np.concatenate([inp["w_group"][i], inp["w_router"][i]], axis=1), 16)
    d["br"] = np.ascontiguousarray(np.broadcast_to(np.concatenate([inp["b_group"][i], inp["b_router"][i]])[None, :], (128, 72))).astype(np.float32)
    d["ident"] = np.eye(128, dtype=np.float32)
    d["tri"] = np.triu(np.ones((128, 128), np.float32), k=1)
    d["ebase"] = np.ascontiguousarray(np.broadcast_to((np.arange(64, dtype=np.float32) * CAP)[None, :], (128, 64)))
    d["wg"] = np.ascontiguousarray(inp["moe_w_gate"][i].reshape(64, 16, 128, FF).transpose(0, 2, 1, 3).reshape(64, 128, 16 * FF))
    d["wu"] = np.ascontiguousarray(inp["moe_w_up"][i].reshape(64, 16, 128, FF).transpose(0, 2, 1, 3).reshape(64, 128, 16 * FF))
    d["wd"] = np.ascontiguousarray(inp["moe_w_down"][i].reshape(64, 4, 128, 2048).transpose(0, 2, 1, 3).reshape(64, 128, 4 * 2048))
    d["gple"] = gvec(inp["norm_ple_g"][i])
    d["wpp"] = out_chunk_major(inp["ple_w_proj"][i], 2)
    d["wpg"] = out_chunk_major(inp["ple_w_gate"][i], 16)
    d["gfin"] = gvec(inp["final_norm_g"])
    return d


def to_fm(a):
    return np.ascontiguousarray(a.T).reshape(a.shape[1] // 128, 128, a.shape[0])


def from_fm(a):
    return np.ascontiguousarray(a.reshape(-1, a.shape[-1]).T)


SEQ = 2048
ROPE_THETA = 500000.0
TWO_PI = float(2 * np.pi)


def emit_sincos(k, ang_ap, d_ang, cos_t, d_cos, sin_t, d_sin, tmp, d_tmp, rows, n):
    k.ts("dve", tmp[0:rows, 0:n], ang_ap, 1.0 / TWO_PI, MAGIC, ALU.mult, ALU.add, reads=[d_ang], writes=[d_tmp])
    k.ts("dve", tmp[0:rows, 0:n], tmp[0:rows, 0:n], MAGIC, None, ALU.subtract, reads=[d_tmp], writes=[d_tmp])
    k.stt("dve", sin_t[0:rows, 0:n], tmp[0:rows, 0:n], -TWO_PI, ang_ap, ALU.mult, ALU.add, reads=[d_tmp, d_ang], writes=[d_sin])
    k.stt("dve", cos_t[0:rows, 0:n], sin_t[0:rows, 0:n], -1.0, sin_t[0:rows, 0:n], ALU.mult, ALU.max, reads=[d_sin], writes=[d_cos])
    k.ts("dve", cos_t[0:rows, 0:n], cos_t[0:rows, 0:n], -1.0, float(np.pi / 2), ALU.mult, ALU.add, reads=[d_cos], writes=[d_cos])
    k.act(cos_t[0:rows, 0:n], cos_t[0:rows, 0:n], AF.Sin, reads=[d_cos], writes=[d_cos], scale=0.999999)
    k.act(sin_t[0:rows, 0:n], sin_t[0:rows, 0:n], AF.Sin, reads=[d_sin], writes=[d_sin], scale=0.999999)


def build_L2(phases=(1, 2, 3, 4), dbg=99):
    nc = bass.Bass("TRN2", target_bir_lowering=False)
    with ExitStack() as st:
        k = K(nc, st)
        P = k.P
        z_d = k.dram_in("z", [ZCH, 128, SEQ])
        pos_d = k.dram_in("pos", [1, SEQ], I32)
        convw_d = k.dram_in("convw", [128, 4, 3])
        ident_d = k.dram_in("ident", [128, 128])
        pswd_d = k.dram_in("pswd", [128, 128])
        pswm_d = k.dram_in("pswm", [128, 128])
        invf_d = k.dram_in("invf", [1, 256])
        mask2_d = k.dram_in("mask2", [128, 256])
        iota_d = k.dram_in("iota", [128, SEQ])
        sprm_d = k.dram_in("sprm", [128, 3 * 14])
        sB_d = k.dram_in("sB", [14, 128, 256])
        sC_d = k.dram_in("sC", [14, 128, 256])
        sd_d = k.dram_in("sd", [128, 4])
        gluw_d = k.dram_in("gluw", [4, 128, 512])
        glub_d = k.dram_in("glub", [128, 4])
        gq_d = k.dram_in("gq", [128, 3])
        gkv_d = k.dram_in("gkv", [128, 2])
        wuq_d = k.dram_in("wuq", [128, 3 * 864])
        wuk_d = k.dram_in("wuk", [128, 2 * 576])
        wuv_d = k.dram_in("wuv", [128, 2 * 576])
        mix_d = k.dram_out("mix", [MIXCH * 128, SEQ])

        ident, d_ident = k.sb([128, 128], F32)
        ones, d_ones = k.sb([128, 128], F32)
        onesb, d_onesb = k.sb([128, 64], BF16)
        mask2, d_mask2 = k.sb([128, 256], BF16)
        posf, d_posf = k.sb([1, SEQ], F32)
        posi, d_posi = k.sb([1, SEQ], I32)
        invf, d_invf = k.sb([1, 256], F32)
        zero_t, d_zero = k.sb([64, SEQ], F32)
        k.dma("act", ident[:], ident_d, writes=[d_ident])
        k.dma("pool", mask2[:], mask2_d, writes=[d_mask2])
        k.dma("act", posi[:], pos_d, writes=[d_posi])
        k.dma("act", invf[:], invf_d, writes=[d_invf])
        k.memset("pool", ones[:], 1.0, writes=[d_ones])
        k.memset("pool", onesb[:], 1.0, writes=[d_onesb])
        k.memset("pool", zero_t[:], 0.0, writes=[d_zero])
        k.copy("dve", posf[:], posi[:], reads=[d_posi], writes=[d_posf])
        d_out = Dep()
        for r0 in (448, 960):
            k.dma("sp", mix_d[r0:r0 + 64, :], zero_t[:], reads=[d_zero], writes=[d_out])

        P.muted = (1 not in phases)
        with ExitStack() as ph:
            k.st = ph
            cw, d_cw = k.sb([128, 4, 3], F32)
            k.dma("act", cw[:], convw_d, writes=[d_cw])
            bufs = [[k.sb([128, SEQ], F32) for _ in range(3)] for _ in range(2)]
            ys = [k.sb([128, SEQ], F32) for _ in range(2)]
            for j in range(4):
                (cb, d_cb), (cc, d_cc), (chh, d_ch) = bufs[j % 2]
                y, d_y = ys[j % 2]
                k.dma("sp", cb[:], z_d[0 + j], writes=[d_cb])
                k.dma("act", cc[:], z_d[4 + j], writes=[d_cc])
                k.dma("sp", chh[:], z_d[8 + j], writes=[d_ch])
                k.tt("dve", cc[:], cc[:], chh[:], ALU.mult, reads=[d_cc, d_ch], writes=[d_cc])
                k.ts("dve", y[:], cc[:], cw[:, j, 2:3], None, ALU.mult, reads=[d_cc, d_cw], writes=[d_y])
                k.stt("dve", y[:, 1:SEQ], cc[:, 0:SEQ - 1], cw[:, j, 1:2], y[:, 1:SEQ], ALU.mult, ALU.add,
                      reads=[d_cc, d_cw, d_y], writes=[d_y])
                k.stt("dve", y[:, 2:SEQ], cc[:, 0:SEQ - 2], cw[:, j, 0:1], y[:, 2:SEQ], ALU.mult, ALU.add,
                      reads=[d_cc, d_cw, d_y], writes=[d_y])
                k.tt("pool", y[:], y[:], cb[:], ALU.mult, reads=[d_cb, d_y], writes=[d_y])
                rows = 128 if j < 3 else 64
                k.dma("sp", mix_d[j * 128:j * 128 + rows, :], y[0:rows, :], reads=[d_y], writes=[d_y, d_out])
            P.barrier()
            P.emit(st)

        P.muted = (2 not in phases)
        with ExitStack() as ph:
            k.st = ph
            su = [k.sb([128, SEQ], F32) for _ in range(4)]
            for c in range(4):
                k.dma("sp" if c % 2 == 0 else "act", su[c][0][:], z_d[12 + c], writes=[su[c][1]])
            iota, d_iota = k.sb([128, SEQ], F32)
            k.dma("act", iota[:], iota_d, writes=[d_iota])
            prm, d_prm = k.sb([128, 42], F32)
            k.dma("act", prm[:], sprm_d, writes=[d_prm])
            sd, d_sd = k.sb([128, 4], F32)
            k.dma("act", sd[:], sd_d, writes=[d_sd])
            glub, d_glub = k.sb([128, 4], F32)
            k.dma("act", glub[:], glub_d, writes=[d_glub])
            def col14():
                return k.sb([128, 14], F32)
            lr, li, ldt = prm[:, 0:14], prm[:, 14:28], prm[:, 28:42]
            (dt, d_dt), (amag, d_amag), (th, d_th), (thr, d_thr) = col14(), col14(), col14(), col14()
            (cs, d_cs), (sn, d_sn), (t0, d_t0), (t1, d_t1) = col14(), col14(), col14(), col14()
            (fre, d_fre), (fim, d_fim), (den, d_den) = col14(), col14(), col14()
            k.act(dt[:], ldt, AF.Exp, reads=[d_prm], writes=[d_dt])
            k.tt("dve", amag[:], lr, dt[:], ALU.mult, reads=[d_prm, d_dt], writes=[d_amag])
            k.act(amag[:], amag[:], AF.Exp, reads=[d_amag], writes=[d_amag])
            k.tt("dve", th[:], li, dt[:], ALU.mult, reads=[d_prm, d_dt], writes=[d_th])
            k.ts("dve", t0[:], th[:], 1.0 / TWO_PI, MAGIC, ALU.mult, ALU.add, reads=[d_th], writes=[d_t0])
            k.ts("dve", t0[:], t0[:], MAGIC, None, ALU.subtract, reads=[d_t0], writes=[d_t0])
            k.stt("dve", thr[:], t0[:], -TWO_PI, th[:], ALU.mult, ALU.add, reads=[d_t0, d_th], writes=[d_thr])
            emit_sincos(k, thr[:], d_thr, cs, d_cs, sn, d_sn, t1, d_t1, 128, 14)
            (nr, d_nr), (ni, d_ni) = col14(), col14()
            k.tt("dve", nr[:], amag[:], cs[:], ALU.mult, reads=[d_amag, d_cs], writes=[d_nr])
            k.ts("dve", nr[:], nr[:], -1.0, None, ALU.add, reads=[d_nr], writes=[d_nr])
            k.tt("dve", ni[:], amag[:], sn[:], ALU.mult, reads=[d_amag, d_sn], writes=[d_ni])
            k.tt("dve", den[:], lr, lr, ALU.mult, reads=[d_prm], writes=[d_den])
            k.tt("dve", t0[:], li, li, ALU.mult, reads=[d_prm, d_t0], writes=[d_t0])
            k.tt("dve", den[:], den[:], t0[:], ALU.add, reads=[d_den, d_t0], writes=[d_den])
            P.op("dve", lambda e: e.reciprocal(den[:], den[:]), [d_den], [d_den])
            k.tt("dve", fre[:], nr[:], lr, ALU.mult, reads=[d_nr, d_prm], writes=[d_fre])
            k.tt("dve", t0[:], ni[:], li, ALU.mult, reads=[d_ni, d_prm, d_t0], writes=[d_t0])
            k.tt("dve", fre[:], fre[:], t0[:], ALU.add, reads=[d_fre, d_t0], writes=[d_fre])
            k.tt("dve", fre[:], fre[:], den[:], ALU.mult, reads=[d_fre, d_den], writes=[d_fre])
            k.tt("dve", fim[:], ni[:], lr, ALU.mult, reads=[d_ni, d_prm], writes=[d_fim])
            k.tt("dve", t0[:], nr[:], li, ALU.mult, reads=[d_nr, d_prm, d_t0], writes=[d_t0])
            k.tt("dve", fim[:], fim[:], t0[:], ALU.subtract, reads=[d_fim, d_t0], writes=[d_fim])
            k.tt("dve", fim[:], fim[:], den[:], ALU.mult, reads=[d_fim, d_den], writes=[d_fim])
            (nfim, d_nfim) = col14()
            k.ts("dve", nfim[:], fim[:], -1.0, None, ALU.mult, reads=[d_fim], writes=[d_nfim])
            (nfre, d_nfre) = col14()
            k.ts("dve", nfre[:], fre[:], -1.0, None, ALU.mult, reads=[d_fre], writes=[d_nfre])

            cosT, d_cosT = k.sb([128, SEQ], F32)
            sinT, d_sinT = k.sb([128, SEQ], F32)
            ang, d_ang = k.sb([128, SEQ], F32)
            tmpA, d_tmpA = k.sb([128, SEQ], F32)
            dec, d_dec = k.sb([128, SEQ], F32)
            wre, d_wre = k.sb([128, SEQ], F32)
            wim, d_wim = k.sb([128, SEQ], F32)
            tb, d_tb = k.sb([128, SEQ], F32)
            Bt = [k.sb([128, 256], F32) for _ in range(2)]
            Ct = [k.sb([128, 256], F32) for _ in range(2)]
            Cp = [k.sb([128, 256], F32) for _ in range(2)]
            pbu = [k.ps([128, 512]) for _ in range(4)]
            py = [k.ps([128, 512]) for _ in range(4)]
            yss = [k.sb([128, SEQ], F32) for _ in range(4)]
            for j in range(14):
                c = j // 4
                B_, d_B = Bt[j % 2]
                C_, d_C = Ct[j % 2]
                Cq, d_Cq = Cp[j % 2]
                k.dma("sp", B_[:], sB_d[j], writes=[d_B])
                k.dma("act", C_[:], sC_d[j], writes=[d_C])
                k.ts("dve", Cq[:, 0:128], C_[:, 0:128], fre[:, j:j + 1], None, ALU.mult, reads=[d_C, d_fre], writes=[d_Cq])
                k.stt("dve", Cq[:, 0:128], C_[:, 128:256], nfim[:, j:j + 1], Cq[:, 0:128], ALU.mult, ALU.add,
                      reads=[d_C, d_nfim, d_Cq], writes=[d_Cq])
                k.ts("dve", Cq[:, 128:256], C_[:, 0:128], nfim[:, j:j + 1], None, ALU.mult, reads=[d_C, d_nfim], writes=[d_Cq])
                k.stt("dve", Cq[:, 128:256], C_[:, 128:256], nfre[:, j:j + 1], Cq[:, 128:256], ALU.mult, ALU.add,
                      reads=[d_C, d_nfre, d_Cq], writes=[d_Cq])
                k.ts("dve", ang[:], iota[:], thr[:, j:j + 1], None, ALU.mult, reads=[d_iota, d_thr], writes=[d_ang])
                emit_sincos(k, ang[:], d_ang, cosT, d_cosT, sinT, d_sinT, tmpA, d_tmpA, 128, SEQ)
                k.ts("dve", dec[:], iota[:], 0.0, amag[:, j:j + 1], ALU.mult, ALU.add, reads=[d_iota, d_amag], writes=[d_dec])
                for hf in range(2):
                    hs = slice(hf * 1024, (hf + 1) * 1024)
                    for t in range(2):
                        tsl = slice(hf * 1024 + t * 512, hf * 1024 + (t + 1) * 512)
                        k.mm(pbu[t][0][:, :], B_[:, 0:128], su[c][0][:, tsl], True, True, reads=[d_B, su[c][1]], writes=[pbu[t][1]])
                        k.mm(pbu[2 + t][0][:, :], B_[:, 128:256], su[c][0][:, tsl], True, True, reads=[d_B, su[c][1]], writes=[pbu[2 + t][1]])
                    for t in range(2):
                        tsl = slice(hf * 1024 + t * 512, hf * 1024 + (t + 1) * 512)
                        bre, d_bre = pbu[t]
                        bim, d_bim = pbu[2 + t]
                        k.tt("dve", wre[:, tsl], cosT[:, tsl], bre[:, :], ALU.mult, reads=[d_cosT, d_bre], writes=[d_wre])
                        k.tt("dve", tb[:, tsl], sinT[:, tsl], bim[:, :], ALU.mult, reads=[d_sinT, d_bim], writes=[d_tb])
                        k.tt("pool", wre[:, tsl], wre[:, tsl], tb[:, tsl], ALU.add, reads=[d_tb, d_wre], writes=[d_wre])
                        k.tt("dve", wim[:, tsl], cosT[:, tsl], bim[:, :], ALU.mult, reads=[d_cosT, d_bim], writes=[d_wim])
                        k.tt("dve", tb[:, tsl], sinT[:, tsl], bre[:, :], ALU.mult, reads=[d_sinT, d_bre, d_wre], writes=[d_tb])
                        k.tt("pool", wim[:, tsl], wim[:, tsl], tb[:, tsl], ALU.subtract, reads=[d_tb, d_wim], writes=[d_wim])
                    for (w_, d_w_) in ((wre, d_wre), (wim, d_wim)):
                        init = 0.0 if hf == 0 else w_[:, 1023:1024]
                        P.op("dve", lambda e, w_=w_, hs=hs, init=init: e.tensor_tensor_scan(w_[:, hs], dec[:, hs], w_[:, hs], init, op0=ALU.mult, op1=ALU.add),
                             [d_dec, d_w_], [d_w_])
                k.tt("dve", tmpA[:], cosT[:], wre[:], ALU.mult, reads=[d_cosT, d_wre], writes=[d_tmpA])
                k.tt("pool", tb[:], sinT[:], wim[:], ALU.mult, reads=[d_sinT, d_wim], writes=[d_tb])
                k.tt("dve", tmpA[:], tmpA[:], tb[:], ALU.subtract, reads=[d_tb, d_tmpA], writes=[d_tmpA])
                k.tt("dve", ang[:], sinT[:], wre[:], ALU.mult, reads=[d_sinT, d_wre], writes=[d_ang])
                k.tt("pool", tb[:], cosT[:], wim[:], ALU.mult, reads=[d_cosT, d_wim, d_tmpA], writes=[d_tb])
                k.tt("dve", ang[:], ang[:], tb[:], ALU.add, reads=[d_tb, d_ang], writes=[d_ang])
                first = (j % 4 == 0)
                last = (j % 4 == 3) or (j == 13)
                for t in range(4):
                    tsl = slice(t * 512, (t + 1) * 512)
                    k.mm(py[t][0][:, :], Cq[:, 0:128], tmpA[:, tsl], first, False, reads=[d_Cq, d_tmpA], writes=[py[t][1]])
                    k.mm(py[t][0][:, :], Cq[:, 128:256], ang[:, tsl], False, last, reads=[d_Cq, d_ang], writes=[py[t][1]])
                if last:
                    ysc, d_ysc = yss[c]
                    for t in range(4):
                        tsl = slice(t * 512, (t + 1) * 512)
                        k.stt("dve", ysc[:, tsl], su[c][0][:, tsl], sd[:, c:c + 1], py[t][0][:, :], ALU.mult, ALU.add,
                              reads=[su[c][1], d_sd, py[t][1]], writes=[d_ysc])
            for c in range(4):
                ysc, d_ysc = yss[c]
                k.tt("dve", tmpA[:], ysc[:], ysc[:], ALU.mult, reads=[d_ysc], writes=[d_tmpA])
                k.ts("dve", tmpA[:], tmpA[:], 0.044715, 1.0, ALU.mult, ALU.add, reads=[d_tmpA], writes=[d_tmpA])
                k.tt("dve", tmpA[:], tmpA[:], ysc[:], ALU.mult, reads=[d_tmpA, d_ysc], writes=[d_tmpA])
                k.act(tmpA[:], tmpA[:], AF.Sigmoid, reads=[d_tmpA], writes=[d_tmpA], scale=1.5957691216057308)
                k.tt("dve", ysc[:], ysc[:], tmpA[:], ALU.mult, reads=[d_tmpA, d_ysc], writes=[d_ysc])
            gws = []
            for c in range(4):
                t_, d_t_ = (wre, d_wre) if c == 0 else (wim, d_wim) if c == 1 else (dec, d_dec) if c == 2 else (tb, d_tb)
                k.dma("sp", t_[:, 0:512], gluw_d[c], writes=[d_t_])
                gws.append((t_, d_t_))
            for m in range(4):
                for t in range(4):
                    tsl = slice(t * 512, (t + 1) * 512)
                    for c in range(4):
                        k.mm(py[t][0][:, :], gws[c][0][:, m * 128:(m + 1) * 128], yss[c][0][:, tsl], c == 0, c == 3,
                             reads=[gws[c][1], yss[c][1]], writes=[py[t][1]])
                    k.act(tmpA[:, tsl], py[t][0][:, :], AF.Sigmoid, reads=[py[t][1]], writes=[d_tmpA], bias=glub[:, m:m + 1])
                k.tt("dve", ang[:], tmpA[:], yss[m][0][:], ALU.mult, reads=[d_tmpA, yss[m][1]], writes=[d_ang])
                rows = 128 if m < 3 else 64
                k.dma("sp", mix_d[512 + m * 128:512 + m * 128 + rows, :], ang[0:rows, :], reads=[d_ang], writes=[d_ang, d_out])
            P.barrier()
            P.emit(st)

        def rope_tables(ph_k, invf_slice, pbanks, ang, d_ang, cosT, d_cosT, sinT, d_sinT, tmp, d_tmp):
            for t in range(4):
                tsl = slice(t * 512, (t + 1) * 512)
                k.mm(pbanks[t][0][:, :], invf_slice, posf[0:1, tsl], True, True, reads=[d_invf, d_posf], writes=[pbanks[t][1]])
                k.copy("act", ang[:, tsl], pbanks[t][0][:, :], reads=[pbanks[t][1]], writes=[d_ang])
            emit_sincos(k, ang[:], d_ang, cosT, d_cosT, sinT, d_sinT, tmp, d_tmp, 128, SEQ)

        P.muted = (3 not in phases)
        with ExitStack() as ph:
            k.st = ph
            psw, d_psw = k.sb([128, 128], F32)
            k.dma("act", psw[:], pswd_d, writes=[d_psw])
            cosT, d_cosT = k.sb([128, SEQ], F32)
            sinT, d_sinT = k.sb([128, SEQ], F32)
            ang, d_ang = k.sb([128, SEQ], F32)
            tmpA, d_tmpA = k.sb([128, SEQ], F32)
            p4 = [k.ps([128, 512]) for _ in range(4)]
            pS = [k.ps([128, 512]) for _ in range(2)]
            pO = [k.ps([128, 512]) for _ in range(2)]
            rope_tables(k, invf[0:1, 0:128], p4, ang, d_ang, cosT, d_cosT, sinT, d_sinT, tmpA, d_tmpA)
            zst = [k.sb([128, SEQ], F32) for _ in range(2)]
            qk = [k.sb([128, SEQ], BF16) for _ in range(10)]
            for ci in range(10):
                zt, d_zt = zst[ci % 2]
                k.dma("sp" if ci % 2 == 0 else "act", zt[:], z_d[16 + ci], writes=[d_zt])
                for t in range(4):
                    tsl = slice(t * 512, (t + 1) * 512)
                    k.mm(p4[t][0][:, :], psw[:], zt[:, tsl], True, True, reads=[d_psw, d_zt], writes=[p4[t][1]])
                    k.tt("dve", tmpA[:, tsl], p4[t][0][:, :], sinT[:, tsl], ALU.mult, reads=[p4[t][1], d_sinT], writes=[d_tmpA])
                k.tt("pool", ang[:], zt[:], cosT[:], ALU.mult, reads=[d_zt, d_cosT], writes=[d_ang])
                k.tt("dve", qk[ci][0][:], ang[:], tmpA[:], ALU.add, reads=[d_ang, d_tmpA], writes=[qk[ci][1]])
            if dbg < 2:
                P.muted = True
            vt = [k.sb([128, 16, 64], BF16) for _ in range(9)]
            DILS = (1, 4, 16)
            def cols(d, m, b, nb=1):
                s0 = m + d * 128 * b
                return slice(s0, s0 + d * (128 * nb - 1) + 1, d) if d > 1 else slice(s0, s0 + 128 * nb)
            for vc in range(5):
                zt, d_zt = zst[vc % 2]
                k.dma("sp" if vc % 2 == 0 else "act", zt[:], z_d[26 + vc], writes=[d_zt])
                for hh in range(2):
                    h = 2 * vc + hh
                    if h >= 9:
                        continue
                    g = h // 3
                    d = DILS[g]
                    nbl = 16 // d
                    r0 = hh * 64
                    for half in range(2):
                        pv, d_pv = p4[(2 * h + half) % 4]
                        for b8 in range(8):
                            blk = half * 8 + b8
                            m, b = blk // nbl, blk % nbl
                            k.mm(pv[:, b8 * 64:(b8 + 1) * 64], zt[r0:r0 + 64, cols(d, m, b)], ident[r0:r0 + 64, r0:r0 + 64], True, True,
                                 reads=[d_zt, d_ident], writes=[d_pv])
                        k.copy("act" if half == 0 else "dve", vt[h][0][:, half * 8:(half + 1) * 8, :],
                               pv[:, :].rearrange("p (a b) -> p a b", b=64), reads=[d_pv], writes=[vt[h][1]])
            if dbg < 3:
                P.muted = True
            oun = [k.sb([64, SEQ], F32) for _ in range(3)]
            dun = [k.sb([64, SEQ], F32) for _ in range(3)]
            pts = [k.sb([128, 256], BF16) for _ in range(3)]
            pti = 0
            si = 0
            oi = 0
            for hg in range(3):
                for g in range(3):
                    h = 3 * g + hg
                    d = DILS[g]
                    nbl = 16 // d
                    r0 = (h % 2) * 64
                    qh, d_qh = qk[h // 2][0][r0:r0 + 64, :], qk[h // 2][1]
                    kh, d_kh = qk[5 + h // 2][0][r0:r0 + 64, :], qk[5 + h // 2][1]
                    for m in range(d):
                        prev = None
                        for b in range(nbl):
                            N = 256 if b < nbl - 1 else 128
                            S_, d_S = pS[si % 2]; si += 1
                            k.mm(S_[:, 0:N], kh[:, cols(d, m, b)], qh[:, cols(d, m, b, N // 128)], True, True, reads=[d_kh, d_qh], writes=[d_S])
                            pt, d_pt = pts[pti % 3]; pti += 1
                            k.act(pt[:, 0:N], S_[:, 0:N], AF.Exp, reads=[d_S], writes=[d_pt], scale=0.125)
                            k.tt("pool", pt[:, 0:N], pt[:, 0:N], mask2[:, 0:N], ALU.mult, reads=[d_mask2, d_pt], writes=[d_pt])
                            hsel = (oi % 2) * 256; oi += 1
                            (OA, d_OA), (OB, d_OB) = pO[0], pO[1]
                            blk = m * nbl + b
                            if prev is not None:
                                pp, d_pp = prev
                                k.mm(OA[0:64, hsel:hsel + 128], vt[h][0][:, blk - 1, :], pp[:, 128:256], True, False, reads=[vt[h][1], d_pp], writes=[d_OA])
                            k.mm(OA[0:64, hsel:hsel + 128], vt[h][0][:, blk, :], pt[:, 0:128], prev is None, True, reads=[vt[h][1], d_pt], writes=[d_OA])
                            if prev is not None:
                                k.mm(OB[0:64, hsel:hsel + 128], onesb[:, 0:64], pp[:, 128:256], True, False, reads=[d_onesb, d_pp], writes=[d_OB])
                            k.mm(OB[0:64, hsel:hsel + 128], onesb[:, 0:64], pt[:, 0:128], prev is None, True, reads=[d_onesb, d_pt], writes=[d_OB])
                            k.copy("act", oun[g][0][:, cols(d, m, b)], OA[0:64, hsel:hsel + 128], reads=[d_OA], writes=[oun[g][1]])
                            k.copy("dve", dun[g][0][:, cols(d, m, b)], OB[0:64, hsel:hsel + 128], reads=[d_OB], writes=[dun[g][1]])
                            prev = (pt, d_pt)
                k.tt("dve", dun[0][0][:], dun[0][0][:], dun[1][0][:], ALU.add, reads=[dun[1][1], dun[0][1]], writes=[dun[0][1]])
                k.tt("dve", dun[0][0][:], dun[0][0][:], dun[2][0][:], ALU.add, reads=[dun[2][1], dun[0][1]], writes=[dun[0][1]])
                P.op("dve", lambda e: e.reciprocal(dun[0][0][:], dun[0][0][:]), [dun[0][1]], [dun[0][1]])
                for g in range(3):
                    h = 3 * g + hg
                    k.tt("dve" if g != 1 else "pool", oun[g][0][:], oun[g][0][:], dun[0][0][:], ALU.mult, reads=[dun[0][1], oun[g][1]], writes=[oun[g][1]])
                    k.dma("sp", mix_d[1024 + h * 64:1024 + (h + 1) * 64, :], oun[g][0][:], reads=[oun[g][1]], writes=[oun[g][1], d_out])
            P.muted = (3 not in phases)
            P.barrier()
            P.emit(st)

        P.muted = (4 not in phases)
        with ExitStack() as ph:
            k.st = ph
            psw, d_psw = k.sb([128, 128], F32)
            k.dma("act", psw[:], pswm_d, writes=[d_psw])
            gq, d_gq = k.sb([128, 3], F32)
            gkv, d_gkv = k.sb([128, 2], F32)
            k.dma("act", gq[:], gq_d, writes=[d_gq])
            k.dma("act", gkv[:], gkv_d, writes=[d_gkv])
            wuq, d_wuq = k.sb([128, 3 * 864], BF16)
            wuk, d_wuk = k.sb([128, 2 * 576], BF16)
            wuv, d_wuv = k.sb([128, 2 * 576], BF16)
            k.dma("pool", wuq[:], wuq_d, writes=[d_wuq])
            k.dma("pool", wuk[:], wuk_d, writes=[d_wuk])
            k.dma("pool", wuv[:], wuv_d, writes=[d_wuv])
            cosT, d_cosT = k.sb([128, SEQ], F32)
            sinT, d_sinT = k.sb([128, SEQ], F32)
            ang, d_ang = k.sb([128, SEQ], F32)
            tmpA, d_tmpA = k.sb([128, SEQ], F32)
            p4 = [k.ps([128, 512]) for _ in range(4)]
            pS = [k.ps([128, 512]) for _ in range(2)]
            pO, d_pO = k.ps([128, 512])
            pD, d_pD = k.ps([128, 512])
            rope_tables(k, invf[0:1, 128:256], p4, ang, d_ang, cosT, d_cosT, sinT, d_sinT, tmpA, d_tmpA)
            zst = [k.sb([128, SEQ], F32) for _ in range(2)]
            rstd, d_rstd = k.sb([128, SEQ], F32)
            cqn = [k.sb([128, SEQ], BF16) for _ in range(3)]
            ckvn = [k.sb([128, SEQ], BF16) for _ in range(2)]

            def norm_group(chunks, nfeat, gt, d_gt, outs):
                n = len(chunks)
                tiles = []
                for i_, zc in enumerate(chunks):
                    zt, d_zt = k.sb([128, SEQ], F32)
                    tiles.append((zt, d_zt))
                    k.dma("sp" if i_ % 2 == 0 else "act", zt[:], z_d[zc], writes=[d_zt])
                    k.act(tmpA[:], zt[:], AF.Square, reads=[d_zt], writes=[d_tmpA])
                    for t in range(4):
                        tsl = slice(t * 512, (t + 1) * 512)
                        k.mm(p4[t][0][:, :], ones[:], tmpA[:, tsl], i_ == 0, i_ == n - 1, reads=[d_ones, d_tmpA], writes=[p4[t][1]])
                for t in range(4):
                    tsl = slice(t * 512, (t + 1) * 512)
                    k.ts("dve", rstd[:, tsl], p4[t][0][:, :], 1.0 / nfeat, NORM_EPS, ALU.mult, ALU.add, reads=[p4[t][1]], writes=[d_rstd])
                k.act(rstd[:], rstd[:], AF.Sqrt, reads=[d_rstd], writes=[d_rstd])
                P.op("dve", lambda e: e.reciprocal(rstd[:], rstd[:]), [d_rstd], [d_rstd])
                for i_, (zt, d_zt) in enumerate(tiles):
                    k.stt("dve", outs[i_][0][:], zt[:], gt[:, i_:i_ + 1], rstd[:], ALU.mult, ALU.mult,
                          reads=[d_zt, d_gt, d_rstd], writes=[outs[i_][1]])

            with ExitStack() as ph2:
                k.st = ph2
                norm_group([31, 32, 33], 384, gq, d_gq, cqn)
                P.barrier(); P.emit(st)
            with ExitStack() as ph2:
                k.st = ph2
                norm_group([34, 35], 256, gkv, d_gkv, ckvn)
                P.barrier(); P.emit(st)
            k.st = ph
            krb, d_krb = k.sb([128, SEQ], BF16)
            zt, d_zt = zst[0]
            k.dma("sp", zt[:], z_d[36], writes=[d_zt])
            for t in range(4):
                tsl = slice(t * 512, (t + 1) * 512)
                k.mm(p4[t][0][:, :], psw[:], zt[:, tsl], True, True, reads=[d_psw, d_zt], writes=[p4[t][1]])
                k.tt("dve", tmpA[:, tsl], p4[t][0][:, :], sinT[:, tsl], ALU.mult, reads=[p4[t][1], d_sinT], writes=[d_tmpA])
            k.tt("pool", ang[:], zt[:], cosT[:], ALU.mult, reads=[d_zt, d_cosT], writes=[d_ang])
            k.tt("dve", krb[:], ang[:], tmpA[:], ALU.add, reads=[d_ang, d_tmpA], writes=[d_krb])
            vt, d_vt = k.sb([128, 16, 576], BF16)
            for blk in range(16):
                bsl = slice(blk * 128, (blk + 1) * 128)
                for hf in range(2):
                    pv, d_pv = p4[(2 * blk + hf) % 4]
                    for kc in range(2):
                        k.mm(pv[:, 0:288], ckvn[kc][0][:, bsl], wuv[:, kc * 576 + hf * 288: kc * 576 + (hf + 1) * 288], kc == 0, kc == 1,
                             reads=[ckvn[kc][1], d_wuv], writes=[d_pv])
                    k.copy("act" if hf == 0 else "dve", vt[:, blk, hf * 288:(hf + 1) * 288], pv[:, 0:288], reads=[d_pv], writes=[d_vt])
            qf, d_qf = k.sb([128, SEQ], F32)
            qbs = [k.sb([128, SEQ], BF16) for _ in range(2)]
            kbs = [k.sb([128, SEQ], BF16) for _ in range(2)]
            ost = [k.sb([64, SEQ], F32) for _ in range(2)]
            rinv, d_rinv = k.sb([64, 512], F32)
            pts = [k.sb([128, 512], BF16) for _ in range(3)]
            pti = 0
            si = 0
            SC = float(1.0 / np.sqrt(96.0))
            for h in range(9):
                qb, d_qb = qbs[h % 2]
                kb_, d_kb = kbs[h % 2]
                for t in range(4):
                    tsl = slice(t * 512, (t + 1) * 512)
                    for kc in range(3):
                        k.mm(p4[t][0][0:96, :], wuq[:, kc * 864 + h * 96: kc * 864 + (h + 1) * 96], cqn[kc][0][:, tsl], kc == 0, kc == 2,
                             reads=[d_wuq, cqn[kc][1]], writes=[p4[t][1]])
                    k.copy("act", qf[0:96, tsl], p4[t][0][0:96, :], reads=[p4[t][1]], writes=[d_qf])
                k.copy("pool", qb[0:64, :], qf[0:64, :], reads=[d_qf], writes=[d_qb])
                for t in range(4):
                    tsl = slice(t * 512, (t + 1) * 512)
                    k.mm(p4[t][0][0:96, :], psw[0:96, 0:96], qf[0:96, tsl], True, True, reads=[d_psw, d_qf], writes=[p4[t][1]])
                    k.tt("dve", tmpA[64:96, tsl], p4[t][0][64:96, :], sinT[64:96, tsl], ALU.mult, reads=[p4[t][1], d_sinT], writes=[d_tmpA])
                k.tt("pool", ang[64:96, :], qf[64:96, :], cosT[64:96, :], ALU.mult, reads=[d_qf, d_cosT], writes=[d_ang])
                k.tt("dve", qb[64:96, :], ang[64:96, :], tmpA[64:96, :], ALU.add, reads=[d_ang, d_tmpA], writes=[d_qb])
                for t in range(4):
                    tsl = slice(t * 512, (t + 1) * 512)
                    for kc in range(2):
                        k.mm(p4[t][0][0:64, :], wuk[:, kc * 576 + h * 64: kc * 576 + (h + 1) * 64], ckvn[kc][0][:, tsl], kc == 0, kc == 1,
                             reads=[d_wuk, ckvn[kc][1]], writes=[p4[t][1]])
                    k.copy("act" if t % 2 == 0 else "dve", kb_[0:64, tsl], p4[t][0][0:64, :], reads=[p4[t][1]], writes=[d_kb])
                k.copy("pool", kb_[64:96, :], krb[64:96, :], reads=[d_krb], writes=[d_kb])
                o_, d_o = ost[h % 2]
                for qt in range(4):
                    nk = 4 * qt + 4
                    for kbi in range(nk):
                        n0 = max(0, kbi - 4 * qt)
                        q0 = qt * 512 + n0 * 128
                        N = 512 - n0 * 128
                        S_, d_S = pS[si % 2]; si += 1
                        k.mm(S_[:, 0:N], kb_[0:96, kbi * 128:(kbi + 1) * 128], qb[0:96, q0:q0 + N], True, True, reads=[d_kb, d_qb], writes=[d_S])
                        pt, d_pt = pts[pti % 3]; pti += 1
                        k.act(pt[:, 0:N], S_[:, 0:N], AF.Exp, reads=[d_S], writes=[d_pt], scale=SC)
                        if kbi >= 4 * qt:
                            k.tt("pool", pt[:, 0:128], pt[:, 0:128], mask2[:, 0:128], ALU.mult, reads=[d_mask2, d_pt], writes=[d_pt])
                        P.op("pe", lambda e, pt=pt, N=N, n0=n0, kbi=kbi, h=h, nk=nk: e.matmul(
                            pO[0:64, n0 * 128:512], vt[:, kbi, h * 64:(h + 1) * 64], pt[:, 0:N], start=(kbi == 0), stop=(kbi == nk - 1), skip_group_check=True),
                            [d_vt, d_pt], [d_pO])
                        P.op("pe", lambda e, pt=pt, N=N, n0=n0, kbi=kbi, nk=nk: e.matmul(
                            pD[0:64, n0 * 128:512], onesb[:, 0:64], pt[:, 0:N], start=(kbi == 0), stop=(kbi == nk - 1), skip_group_check=True),
                            [d_onesb, d_pt], [d_pD])
                    P.op("dve", lambda e: e.reciprocal(rinv[:], pD[0:64, :]), [d_pD], [d_rinv])
                    k.tt("dve", o_[:, qt * 512:(qt + 1) * 512], pO[0:64, :], rinv[:], ALU.mult, reads=[d_pO, d_rinv], writes=[d_o])
                k.dma("sp", mix_d[1600 + h * 64:1600 + (h + 1) * 64, :], o_[:], reads=[d_o], writes=[d_o, d_out])
            P.barrier()
            P.emit(st)
        P.muted = False
        k.st = st
        return nc


def prep_L2_weights(inp, i):
    d = {}
    cw = np.zeros((512, 3), np.float32)
    cw[:448] = inp["conv_w"][i].T
    d["convw"] = np.ascontiguousarray(cw.reshape(4, 128, 3).transpose(1, 0, 2))
    d["ident"] = np.eye(128, dtype=np.float32)
    pswd = np.zeros((128, 128), np.float32)
    for off in (0, 64):
        for m in range(8):
            pswd[off + m + 8, off + m] = -1.0
            pswd[off + m, off + m + 8] = 1.0
    d["pswd"] = pswd
    pswm = np.zeros((128, 128), np.float32)
    for m in range(16):
        pswm[80 + m, 64 + m] = -1.0
        pswm[64 + m, 80 + m] = 1.0
    d["pswm"] = pswm
    invf = np.zeros((1, 256), np.float32)
    fd = (ROPE_THETA ** (-np.arange(0, 16, 2, dtype=np.float32) / 16)).astype(np.float32)
    fm = (ROPE_THETA ** (-np.arange(0, 32, 2, dtype=np.float32) / 32)).astype(np.float32)
    for off in (0, 64):
        invf[0, off:off + 8] = fd
        invf[0, off + 8:off + 16] = fd
    invf[0, 128 + 64:128 + 80] = fm
    invf[0, 128 + 80:128 + 96] = fm
    d["invf"] = invf
    tri = np.triu(np.ones((128, 128), np.float32))
    d["mask2"] = np.ascontiguousarray(np.concatenate([tri, tri.T], axis=1))
    d["iota"] = np.ascontiguousarray(np.broadcast_to(np.arange(SEQ, dtype=np.float32)[None, :], (128, SEQ)))
    lr, li, ldt = inp["ssm_lam_re"][i], inp["ssm_lam_im"][i], inp["ssm_log_dt"][i]
    prm = np.zeros((128, 42), np.float32)
    sB = np.zeros((14, 128, 256), np.float32)
    sC = np.zeros((14, 128, 256), np.float32)
    for j in range(14):
        for gl in range(2):
            g = 2 * j + gl
            prm[gl * 64:(gl + 1) * 64, j] = lr[g]
            prm[gl * 64:(gl + 1) * 64, 14 + j] = li[g]
            prm[gl * 64:(gl + 1) * 64, 28 + j] = ldt[g]
            r0 = (g % 8) * 16
            sB[j, r0:r0 + 16, gl * 64:(gl + 1) * 64] = inp["ssm_b_re"][i][g].T
            sB[j, r0:r0 + 16, 128 + gl * 64:128 + (gl + 1) * 64] = inp["ssm_b_im"][i][g].T
            sC[j, gl * 64:(gl + 1) * 64, r0:r0 + 16] = inp["ssm_c_re"][i][g].T
            sC[j, gl * 64:(gl + 1) * 64, 128 + r0:128 + r0 + 16] = inp["ssm_c_im"][i][g].T
    d["sprm"], d["sB"], d["sC"] = prm, sB, sC
    sdp = np.zeros(512, np.float32); sdp[:448] = inp["ssm_d"][i]
    d["sd"] = gvec(sdp)
    gw = np.zeros((512, 512), np.float32); gw[:448, :448] = inp["ssm_glu_w"][i]
    d["gluw"] = np.ascontiguousarray(gw.reshape(4, 128, 512))
    gb = np.zeros(512, np.float32); gb[:448] = inp["ssm_glu_b"][i]
    d["glub"] = gvec(gb)
    d["gq"] = gvec(inp["mla_q_norm_g"][i])
    d["gkv"] = gvec(inp["mla_kv_norm_g"][i])
    d["wuq"] = chunk_major(inp["mla_w_uq"][i], 3)
    wkv = inp["mla_w_ukv"][i].reshape(256, 9, 128)
    d["wuk"] = chunk_major(np.ascontiguousarray(wkv[:, :, 0:64]).reshape(256, 576), 2)
    d["wuv"] = chunk_major(np.ascontiguousarray(wkv[:, :, 64:128]).reshape(256, 576), 2)
    return d


_NC = {}


def _prog(name, fn):
    if name not in _NC:
        _NC[name] = fn()
    return _NC[name]


def _run(name, fn, maps):
    nc = _prog(name, fn)
    res = run_bass_kernel_spmd(nc, maps, core_ids=list(range(8)))
    return res.results


def prep_L3a_weights(inp, i):
    rm = mix_rowmap()
    valid = rm >= 0
    wo = np.zeros((MIXCH * 128, 2048), np.float32)
    wo[valid] = inp["w_out"][i][rm[valid]]
    d = {}
    d["wout"] = out_chunk_major(wo, MIXCH)
    d["gffn"] = gvec(inp["norm_ffn_g"][i])
    d["wr"] = chunk_major(np.concatenate([inp["w_group"][i], inp["w_router"][i]], axis=1), 16)
    d["br"] = np.ascontiguousarray(np.broadcast_to(np.concatenate([inp["b_group"][i], inp["b_router"][i]])[None, :], (128, 72))).astype(np.float32)
    d["ident"] = np.eye(128, dtype=np.float32)
    d["tri"] = np.triu(np.ones((128, 128), np.float32), k=1)
    d["ebase"] = np.ascontiguousarray(np.broadcast_to((np.arange(64, dtype=np.float32) * CAP)[None, :], (128, 64)))
    return d


def prep_L3c_weights(inp, i):
    d = {}
    d["ident"] = np.eye(128, dtype=np.float32)
    d["gple"] = gvec(inp["norm_ple_g"][i])
    d["wpp"] = out_chunk_major(inp["ple_w_proj"][i], 2)
    d["wpg"] = out_chunk_major(inp["ple_w_gate"][i], 16)
    d["gfin"] = gvec(inp["final_norm_g"])
    return d


def kernel(**inp):
    inp = {k_: np.asarray(v) for k_, v in inp.items()}
    x = inp["x"].astype(np.float32, copy=False)
    positions = inp["positions"].astype(np.int32, copy=False)
    hT = [to_fm(x.reshape(8, NTOK, D_MODEL)[c]) for c in range(8)]
    cm = z_colmap()
    hn = None
    for i in range(2):
        w1, g1 = prep_L1_weights(inp["w_in"][i], inp["norm_mix_g"][i])
        r1 = _run("L1", build_L1, [{"hT": hT[c], "g": g1, "w": w1} for c in range(8)])
        del w1
        W2 = prep_L2_weights(inp, i)
        zb = [np.ascontiguousarray(np.concatenate([r1[2 * b]["z"], r1[2 * b + 1]["z"]], axis=2)) for b in range(4)]
        del r1
        r2 = _run("L2", build_L2, [dict(W2, z=zb[c % 4], pos=np.ascontiguousarray(positions[c % 4][None])) for c in range(8)])
        del zb
        mix = [np.ascontiguousarray(r2[c // 2]["mix"][:, (c % 2) * NTOK:(c % 2 + 1) * NTOK]).reshape(MIXCH, 128, NTOK) for c in range(8)]
        del r2
        W3a = prep_L3a_weights(inp, i)
        r3a = _run("L3a", build_L3a, [dict(W3a, hT=hT[c], mix=mix[c]) for c in range(8)])
        del mix
        xall = np.stack([r["xbuf"].reshape(8, 8 * CAP, 2048) for r in r3a])
        xin = [np.ascontiguousarray(xall[:, g].reshape(8, 8, CAP, 2048)).reshape(NEXP * CAP, 2048) for g in range(8)]
        del xall
        ident = np.eye(128, dtype=np.float32)
        r3b = _run("L3b", build_L3b, [{"xin": xin[g], "ident": ident,
                                       "wg": np.ascontiguousarray(inp["moe_w_gate"][i][8 * g:8 * g + 8]),
                                       "wu": np.ascontiguousarray(inp["moe_w_up"][i][8 * g:8 * g + 8]),
                                       "wd": np.ascontiguousarray(inp["moe_w_down"][i][8 * g:8 * g + 8])} for g in range(8)])
        del xin
        yall = np.stack([r["yout"].reshape(8, 8 * CAP, 2048) for r in r3b])
        del r3b
        ybuf = [np.ascontiguousarray(yall[:, s_]).reshape(NEXP * CAP, 2048) for s_ in range(8)]
        del yall
        W3c = prep_L3c_weights(inp, i)
        p_i = inp["p"][i].reshape(8, NTOK, 256)
        r3c = _run("L3c", build_L3c, [dict(W3c, hT=r3a[c]["hout"], ybuf=ybuf[c], slot=r3a[c]["slot"], gates=r3a[c]["gates"],
                                           pT=to_fm(p_i[c])) for c in range(8)])
        del r3a, ybuf
        hT = [r["hout"] for r in r3c]
        hn = [r["hnout"] for r in r3c]
    out = np.stack([from_fm(hn[c]) for c in range(8)]).reshape(4, SEQ, D_MODEL)
    return out.astype(np.float32)
```
